# Optimizing a Trainium2 kernel written in Bass

```python
import math
import jax, jax.numpy as jnp
from jax import lax
import numpy as np

D_MODEL = 1024
BATCH = 8
SEQ = 2048
DEPTH = 1
DEC_BATCH = 32
DEC_SEQ = 1
PAST_LEN = 8192
PAGE_SIZE = 128

H_A = 8
DH_A = 64
W_A = H_A * DH_A
MOBA_BLOCK = 256
MOBA_TOPK = 3
Q_BLOCK = 16
N_BUCKETS = 32
MAX_DISTANCE = 128
H_R = 8
DH_R = 64
W_R = H_R * DH_R
LORA_W = 64
LORA_A = 64
LORA_G = 160
RW_COLS = 3 * W_R + LORA_W + LORA_A + LORA_G
IN_COLS = 3 * W_A + RW_COLS
N_GROUPS = 4
EXPERTS_PER_GROUP = 4
N_EXPERTS = N_GROUPS * EXPERTS_PER_GROUP
D_EXPERT = 256
EXPERT_TOPK = 2
PLE_DIM = 256
RMS_EPS = 1e-6
GN_EPS = 64e-5
NEG = -1e30

kernel_name = "hymba_moba_rwkv7_hmoe_decode_step"


def rms_norm(x, g):
    xf = x.astype(jnp.float32)
    y = xf * lax.rsqrt(jnp.mean(xf * xf, axis=-1, keepdims=True) + RMS_EPS)
    return (y * g.astype(jnp.float32)).astype(x.dtype)


def rel_bucket(dist):
    n = jnp.maximum(dist, 0)
    max_exact = N_BUCKETS // 2
    nf = jnp.maximum(n, 1).astype(jnp.float32)
    large = max_exact + (jnp.log(nf / max_exact) / math.log(MAX_DISTANCE / max_exact)
                         * (N_BUCKETS - max_exact)).astype(jnp.int32)
    large = jnp.minimum(large, N_BUCKETS - 1)
    return jnp.where(n < max_exact, n, large)


def moba_attention(q, k_all, v_all, q_pos0, rel_bias):
    B, Sq = q.shape[0], q.shape[1]
    L = k_all.shape[1]
    n_blk = -(-L // MOBA_BLOCK)
    pad = n_blk * MOBA_BLOCK - L
    padw = ((0, 0), (0, pad), (0, 0), (0, 0))
    k_blk = jnp.pad(k_all, padw).reshape(B, n_blk, MOBA_BLOCK, H_A, DH_A).transpose(0, 3, 1, 2, 4)
    v_blk = jnp.pad(v_all, padw).reshape(B, n_blk, MOBA_BLOCK, H_A, DH_A).transpose(0, 3, 1, 2, 4)
    k_mean = jnp.mean(k_blk.astype(jnp.float32), axis=3)
    topk = min(MOBA_TOPK, n_blk)
    chunk = math.gcd(Sq, Q_BLOCK)
    n_chunks = Sq // chunk
    qh = q.transpose(0, 2, 1, 3)
    q_chunks = qh.reshape(B, H_A, n_chunks, chunk, DH_A).transpose(2, 0, 1, 3, 4)
    pos_chunks = (q_pos0 + jnp.arange(Sq, dtype=jnp.int32)).reshape(n_chunks, chunk)
    bias_hb = rel_bias.astype(jnp.float32).T
    b_ix = jnp.arange(B)[:, None, None, None]
    h_ix = jnp.arange(H_A)[None, :, None, None]
    scale = DH_A ** -0.5

    def one_chunk(args):
        qc, pos = args
        own = pos // MOBA_BLOCK
        gate = jnp.einsum('bhcd,bhnd->bhcn', qc.astype(jnp.float32), k_mean)
        fully_past = jnp.arange(n_blk)[None, :] < own[:, None]
        gate = jnp.where(fully_past, gate, NEG)
        _, past_idx = lax.top_k(gate, topk)
        own_b = jnp.broadcast_to(own[None, None, :, None], (B, H_A, chunk, 1)).astype(past_idx.dtype)
        idx = jnp.concatenate([past_idx, own_b], axis=-1)
        slot_ok = jnp.concatenate([jnp.arange(topk)[None, :] < own[:, None],
                                   jnp.ones((chunk, 1), dtype=bool)], axis=-1)
        kg = k_blk[b_ix, h_ix, idx]
        vg = v_blk[b_ix, h_ix, idx]
        s = jnp.einsum('bhcd,bhcnkd->bhcnk', qc, kg).astype(jnp.float32) * scale
        kpos = idx[..., None] * MOBA_BLOCK + jnp.arange(MOBA_BLOCK)
        dist = pos[None, None, :, None, None] - kpos
        s = s + bias_hb[h_ix[..., None], rel_bucket(dist)]
        ok = slot_ok[None, None, :, :, None] & (dist >= 0)
        s = jnp.where(ok, s, NEG)
        pr = jax.nn.softmax(s.reshape(B, H_A, chunk, -1), axis=-1).reshape(s.shape)
        return jnp.einsum('bhcnk,bhcnkd->bhcd', pr.astype(vg.dtype), vg)

    out = lax.map(one_chunk, (q_chunks, pos_chunks))
    return out.transpose(1, 0, 3, 2, 4).reshape(B, Sq, W_A)


def rwkv7_time_mix(z, z_prev, wkv0, shift_mu, w0, w2, a0, a2, g2, k_k, k_a, r_k, lnx_g, lnx_b):
    f32 = jnp.float32
    B, S, _ = z.shape
    z_shift = jnp.concatenate([z_prev[:, None, :].astype(z.dtype), z[:, :-1]], axis=1)
    zs = z + (z_shift - z) * shift_mu
    o1, o2, o3 = W_R, 2 * W_R, 3 * W_R
    o4 = o3 + LORA_W
    o5 = o4 + LORA_A
    r, k, v = zs[..., :o1], zs[..., o1:o2], zs[..., o2:o3]
    xw, xa, xg = zs[..., o3:o4], zs[..., o4:o5], zs[..., o5:]
    w = -jax.nn.softplus(-(w0 + jnp.tanh(xw) @ w2).astype(f32)) - 0.5
    decay = jnp.exp(-jnp.exp(w))
    a = jax.nn.sigmoid((a0 + xa @ a2).astype(f32))
    g = jax.nn.sigmoid(xg) @ g2
    heads = lambda t: t.astype(f32).reshape(B, S, H_R, DH_R)
    kk = heads(k * k_k)
    kk = kk * lax.rsqrt(jnp.maximum(jnp.sum(kk * kk, axis=-1, keepdims=True), 1e-24))
    rh, vh, ah, dh_ = heads(r), heads(v), heads(a), heads(decay)
    kh = heads(k) * (1.0 + (ah - 1.0) * k_a.astype(f32).reshape(H_R, DH_R))

    def step(state, inp):
        r_t, d_t, k_t, v_t, kk_t, a_t = inp
        sa = jnp.einsum('bhij,bhj->bhi', state, -kk_t)
        state = (state * d_t[:, :, None, :] + sa[..., None] * (kk_t * a_t)[:, :, None, :]
                 + v_t[..., None] * k_t[:, :, None, :])
        return state, jnp.einsum('bhij,bhj->bhi', state, r_t)

    tm = lambda t: jnp.swapaxes(t, 0, 1)
    s_fin, y = lax.scan(step, wkv0.astype(f32), (tm(rh), tm(dh_), tm(kh), tm(vh), tm(kk), tm(ah)))
    y = tm(y)
    mu = jnp.mean(y, axis=-1, keepdims=True)
    var = jnp.mean(jnp.square(y - mu), axis=-1, keepdims=True)
    yn = ((y - mu) * lax.rsqrt(var + GN_EPS)).reshape(B, S, W_R) * lnx_g.astype(f32) + lnx_b.astype(f32)
    bonus = (jnp.sum(rh * kh * r_k.astype(f32), axis=-1, keepdims=True) * vh).reshape(B, S, W_R)
    out = ((yn + bonus) * g.astype(f32)).astype(z.dtype)
    return out, s_fin, z[:, -1]


def hier_moe(h, w_rg, b_rg, w_re, b_re, w_eg, w_eu, w_ed):
    B, S, D = h.shape
    t = h.reshape(-1, D)
    gp = jax.nn.softmax((t @ w_rg + b_rg).astype(jnp.float32), axis=-1)
    p_grp, grp = lax.top_k(gp, 1)
    el = (t @ w_re + b_re).astype(jnp.float32).reshape(-1, N_GROUPS, EXPERTS_PER_GROUP)
    el = jnp.take_along_axis(el, grp[:, :, None], axis=1)[:, 0]
    ep = jax.nn.softmax(el, axis=-1)
    p_e, e_loc = lax.top_k(ep, EXPERT_TOPK)
    p_e = p_e / jnp.sum(p_e, axis=-1, keepdims=True)
    e_id = grp * EXPERTS_PER_GROUP + e_loc
    comb = jnp.sum(jax.nn.one_hot(e_id, N_EXPERTS, dtype=jnp.float32) * (p_grp * p_e)[..., None], axis=1)
    hg = jnp.einsum('td,edf->tef', t, w_eg)
    hu = jnp.einsum('td,edf->tef', t, w_eu)
    act = jax.nn.silu(hg) * hu * comb[..., None].astype(t.dtype)
    return jnp.einsum('tef,efd->td', act, w_ed).reshape(B, S, D)


def decoder_layer(x, p_l, past_k, past_v, z_prev, wkv0, q_pos0,
                  norm1_g, w_in, rel_bias, shift_mu, w0, w2, a0, a2, g2, k_k, k_a, r_k,
                  lnx_g, lnx_b, w_out, norm2_g, w_rg, b_rg, w_re, b_re, w_eg, w_eu, w_ed,
                  w_ple, w_pleg):
    B, S, _ = x.shape
    h = rms_norm(x, norm1_g)
    proj = h @ w_in
    q = proj[..., :W_A].reshape(B, S, H_A, DH_A)
    k = proj[..., W_A:2 * W_A].reshape(B, S, H_A, DH_A)
    v = proj[..., 2 * W_A:3 * W_A].reshape(B, S, H_A, DH_A)
    z = proj[..., 3 * W_A:]
    if past_k is None:
        k_all, v_all = k, v
    else:
        k_all = jnp.concatenate([past_k.astype(k.dtype), k], axis=1)
        v_all = jnp.concatenate([past_v.astype(v.dtype), v], axis=1)
    att = moba_attention(q, k_all, v_all, q_pos0, rel_bias)
    rw, wkv_new, shift_new = rwkv7_time_mix(z, z_prev, wkv0, shift_mu, w0, w2, a0, a2, g2,
                                            k_k, k_a, r_k, lnx_g, lnx_b)
    x = x + jnp.concatenate([att, rw.astype(att.dtype)], axis=-1) @ w_out
    x = x + hier_moe(rms_norm(x, norm2_g), w_rg, b_rg, w_re, b_re, w_eg, w_eu, w_ed)
    x = x + (p_l @ w_ple) * jax.nn.sigmoid(x @ w_pleg)
    return x, k, v, wkv_new, shift_new


def setup_inputs(seed: int = 0) -> dict:
    key = jax.random.key(seed)
    ks = iter(jax.random.split(key, 48))
    nrm = lambda shape, s=1.0: jax.random.normal(next(ks), shape, jnp.float32) * s
    n_pages = PAST_LEN // PAGE_SIZE
    n_used = DEC_BATCH * n_pages
    n_pool = n_used + max(1, n_used // 4)
    perm = jax.random.permutation(next(ks), n_pool)
    page_table = perm[:n_used].reshape(DEC_BATCH, n_pages).astype(jnp.int32)
    return {
        "x_prompt": nrm((BATCH, SEQ, D_MODEL)),
        "x_sample": nrm((DEC_BATCH, DEC_SEQ, D_MODEL)),
        "p_prompt": nrm((DEPTH, BATCH, SEQ, PLE_DIM)),
        "p_sample": nrm((DEPTH, DEC_BATCH, DEC_SEQ, PLE_DIM)),
        "cache_k": nrm((DEPTH, n_pool, PAGE_SIZE, H_A, DH_A)),
        "cache_v": nrm((DEPTH, n_pool, PAGE_SIZE, H_A, DH_A)),
        "page_table": page_table,
        "state_wkv": nrm((DEPTH, DEC_BATCH, H_R, DH_R, DH_R), 0.3),
        "state_shift": nrm((DEPTH, DEC_BATCH, RW_COLS)),
        "norm1_g": 1.0 + nrm((DEPTH, D_MODEL), 0.02),
        "w_in": nrm((DEPTH, D_MODEL, IN_COLS), D_MODEL ** -0.5),
        "rel_bias": nrm((N_BUCKETS, H_A), 0.5),
        "shift_mu": jax.random.uniform(next(ks), (DEPTH, RW_COLS), jnp.float32),
        "w0": nrm((DEPTH, W_R), 0.5),
        "w2": nrm((DEPTH, LORA_W, W_R), 0.5 * LORA_W ** -0.5),
        "a0": nrm((DEPTH, W_R), 0.5),
        "a2": nrm((DEPTH, LORA_A, W_R), 0.5 * LORA_A ** -0.5),
        "g2": nrm((DEPTH, LORA_G, W_R), LORA_G ** -0.5),
        "k_k": 0.85 + nrm((DEPTH, W_R), 0.1),
        "k_a": 1.0 + nrm((DEPTH, W_R), 0.1),
        "r_k": nrm((DEPTH, H_R, DH_R), 0.1),
        "lnx_g": 1.0 + nrm((DEPTH, W_R), 0.02),
        "lnx_b": nrm((DEPTH, W_R), 0.02),
        "w_out": nrm((DEPTH, W_A + W_R, D_MODEL), (W_A + W_R) ** -0.5),
        "norm2_g": 1.0 + nrm((DEPTH, D_MODEL), 0.02),
        "w_rg": nrm((DEPTH, D_MODEL, N_GROUPS), D_MODEL ** -0.5),
        "b_rg": nrm((DEPTH, N_GROUPS), 0.01),
        "w_re": nrm((DEPTH, D_MODEL, N_EXPERTS), D_MODEL ** -0.5),
        "b_re": nrm((DEPTH, N_EXPERTS), 0.01),
        "w_eg": nrm((DEPTH, N_EXPERTS, D_MODEL, D_EXPERT), D_MODEL ** -0.5),
        "w_eu": nrm((DEPTH, N_EXPERTS, D_MODEL, D_EXPERT), D_MODEL ** -0.5),
        "w_ed": nrm((DEPTH, N_EXPERTS, D_EXPERT, D_MODEL), D_EXPERT ** -0.5),
        "w_ple": nrm((DEPTH, PLE_DIM, D_MODEL), PLE_DIM ** -0.5),
        "w_pleg": nrm((DEPTH, D_MODEL, D_MODEL), D_MODEL ** -0.5),
        "final_g": 1.0 + nrm((D_MODEL,), 0.02),
    }


def reference(x_prompt, x_sample, p_prompt, p_sample, cache_k, cache_v, page_table, state_wkv,
              state_shift, norm1_g, w_in, rel_bias, shift_mu, w0, w2, a0, a2, g2, k_k, k_a, r_k,
              lnx_g, lnx_b, w_out, norm2_g, w_rg, b_rg, w_re, b_re, w_eg, w_eu, w_ed, w_ple,
              w_pleg, final_g):
    n_prompt = x_prompt.shape[0]
    n_seq = x_sample.shape[0]
    n_pages = page_table.shape[1]
    past_len = n_pages * PAGE_SIZE
    xp, xs = x_prompt, x_sample
    kp_l, vp_l, ks_l, vs_l, wp_l, ws_l, sp_l, ss_l = [], [], [], [], [], [], [], []
    for l in range(DEPTH):
        lw = (norm1_g[l], w_in[l], rel_bias, shift_mu[l], w0[l], w2[l], a0[l], a2[l], g2[l],
              k_k[l], k_a[l], r_k[l], lnx_g[l], lnx_b[l], w_out[l], norm2_g[l], w_rg[l], b_rg[l],
              w_re[l], b_re[l], w_eg[l], w_eu[l], w_ed[l], w_ple[l], w_pleg[l])
        xp, kp, vp, wp, sp = decoder_layer(
            xp, p_prompt[l], None, None,
            jnp.zeros((n_prompt, RW_COLS), xp.dtype),
            jnp.zeros((n_prompt, H_R, DH_R, DH_R), jnp.float32), 0, *lw)
        pk = cache_k[l][page_table].reshape(n_seq, past_len, H_A, DH_A)
        pv = cache_v[l][page_table].reshape(n_seq, past_len, H_A, DH_A)
        xs, ksn, vsn, wsn, ssn = decoder_layer(
            xs, p_sample[l], pk, pv, state_shift[l], state_wkv[l], past_len, *lw)
        kp_l.append(kp); vp_l.append(vp); ks_l.append(ksn); vs_l.append(vsn)
        wp_l.append(wp); ws_l.append(wsn); sp_l.append(sp); ss_l.append(ssn)
    y_prompt = rms_norm(xp, final_g)
    y_sample = rms_norm(xs, final_g)
    return (y_prompt, y_sample, jnp.stack(kp_l), jnp.stack(vp_l), jnp.stack(ks_l), jnp.stack(vs_l),
            jnp.stack(wp_l), jnp.stack(ws_l), jnp.stack(sp_l), jnp.stack(ss_l))
```

```python
import numpy as np
import concourse.bass as bass
import concourse.mybir as mybir
from concourse.bass_utils import run_bass_kernel_spmd
from contextlib import ExitStack

F32 = mybir.dt.float32
BF16 = mybir.dt.bfloat16
I32 = mybir.dt.int32
AF = mybir.ActivationFunctionType
ALU = mybir.AluOpType
AX = mybir.AxisListType

NDS = 48
NDP = 16


class Buf:
    __slots__ = ("ap", "w", "r", "name")

    def __init__(self, ap, name=""):
        self.ap = ap
        self.w = None
        self.r = []
        self.name = name

    def __getitem__(self, k):
        return self.ap[k]


class Prog:
    ENGS = ("pe", "dve", "act", "pool", "sp")

    def __init__(self, nc, es):
        self.nc = nc
        self.es = es
        self.q = {e: [] for e in self.ENGS}
        self.cnt = {e: 0 for e in self.ENGS}
        self.sem = {e: es.enter_context(nc.semaphore("s_" + e)) for e in self.ENGS}
        self.dsem = [es.enter_context(nc.semaphore("d%d" % i)) for i in range(NDS + NDP)]
        self.dcnt = [0] * (NDS + NDP)
        self.di = 0
        self.dpi = 0
        self.seen = {e: {} for e in self.ENGS}
        self.nalloc = 0

    def sb(self, name, shape, dt):
        t = self.es.enter_context(self.nc.sbuf_tensor(name, list(shape), dt))
        return Buf(t.ap(), name)

    def ps(self, name, shape, dt=F32):
        t = self.es.enter_context(self.nc.psum_tensor(name, list(shape), dt))
        return Buf(t.ap(), name)

    def view(self, ap, name=""):
        return Buf(ap, name)

    def _deps(self, tok, eng, reads, writes):
        waits = []
        for b in reads:
            if b.w is not None:
                waits.append((b.w, True))
        for b in writes:
            if b.w is not None:
                waits.append((b.w, False))
            for t in b.r:
                waits.append((t, False))
        for b in reads:
            b.r.append(tok)
        for b in writes:
            b.w = tok
            b.r = []
        out = []
        for t, raw in waits:
            if t[0] == "c" and t[1] == eng and tok[0] == "c" and eng == "pe":
                continue
            out.append(t)
        return out

    def op(self, eng, fn, reads=(), writes=()):
        idx = self.cnt[eng] + 1
        self.cnt[eng] = idx
        tok = ("c", eng, idx)
        waits = self._deps(tok, eng, reads, writes)
        self.q[eng].append((waits, fn, None))
        return tok

    def dma(self, eng, fn, reads=(), writes=()):
        if eng == "pool":
            slot = NDS + self.dpi % NDP
            self.dpi += 1
        else:
            slot = self.di % NDS
            self.di += 1
        prev = self.dcnt[slot]
        self.dcnt[slot] = prev + 1
        tok = ("d", slot, prev + 1)
        waits = self._deps(tok, eng, reads, writes)
        if prev > 0:
            waits.append(("d", slot, prev))
        self.q[eng].append((waits, fn, slot))
        return tok

    def _semval(self, t):
        if t[0] == "c":
            return self.sem[t[1]], t[2], ("c", t[1])
        return self.dsem[t[1]], 16 * t[2], ("d", t[1])

    def flush(self, final=False):
        nc = self.nc
        if final:
            fin = [("d", s, c) for s, c in enumerate(self.dcnt) if c > 0]
            self.q["sp"].append((fin, None, None))
        else:
            toks = [("c", e, self.cnt[e]) for e in self.ENGS if self.cnt[e] > 0]
            toks += [("d", s, c) for s, c in enumerate(self.dcnt) if c > 0]
            for e in self.ENGS:
                self.q[e].append((list(toks), None, None))
        qs = self.q
        self.q = {e: [] for e in self.ENGS}
        with nc.Block() as block:
            def run(engname):
                def body(e):
                    seen = self.seen[engname]
                    for waits, fn, slot in qs[engname]:
                        for t in waits:
                            sem, val, key = self._semval(t)
                            if seen.get(key, 0) >= val:
                                continue
                            seen[key] = val
                            e.wait_ge(sem, val)
                        if fn is None:
                            continue
                        ins = fn(e)
                        if slot is None:
                            ins.then_inc(self.sem[engname], 1)
                        else:
                            ins.then_inc(self.dsem[slot], 16)
                return body
            block.tensor(run("pe"))
            block.vector(run("dve"))
            block.scalar(run("act"))
            block.gpsimd(run("pool"))
            block.sync(run("sp"))

    def emit(self):
        self.flush(final=True)


D_MODEL = 1024
SEQ = 2048
NTILE = 16
NT_ALL = 18
W_A = 512
W_R = 512
RW_COLS = 1824
IN_COLS = 3360
N_EXP = 16
D_EXP = 256
PLE_DIM = 256
N_PAGES = 64
SMAP = {16: ((0, 0), (1, 32), (2, 64)), 17: ((3, 0),)}
SPOS = {0: 2048 + 0, 1: 2048 + 32, 2: 2048 + 64, 3: 2048 + 128}
GROUPS = [(i, 1) for i in range(18)]
NEGB = -240000.0


def _rel_bucket_np(dist):
    n = np.maximum(dist, 0)
    nf = np.maximum(n, 1).astype(np.float32)
    large = 16 + (np.log(nf / 16) / np.log(np.float32(128 / 16)) * 16).astype(np.int32)
    large = np.minimum(large, 31)
    return np.where(n < 16, n, large)


def host_consts():
    c = {}
    c["c_ident"] = np.eye(128, dtype=np.float32)
    r = np.arange(128)[:, None]
    q = np.arange(128)[None, :]
    m = np.zeros((128, 3, 128), np.float32)
    m[:, 0, :] = (r < q)
    m[:, 1, :] = (r <= q)
    m[:, 2, :] = (r > q)
    c["c_masks"] = m
    c["c_bones"] = ((r // 64) == (q // 64)).astype(np.float32)
    ind = np.zeros((128, 2), np.float32)
    ind[:64, 0] = 1
    ind[64:, 1] = 1
    c["c_ind"] = ind
    cc = np.arange(384)
    oh = np.zeros((32, 384), np.float32)
    bk = _rel_bucket_np(cc - 127)
    for i in range(384):
        if cc[i] >= 127:
            oh[bk[i], i] = 1.0
    c["c_oh"] = oh
    negf = np.zeros((128, 384), np.float32)
    negf[:, :127] = NEGB
    c["c_negf"] = negf
    ohs = np.zeros((32, 128), np.float32)
    bks = _rel_bucket_np(128 - np.arange(128))
    ohs[bks, np.arange(128)] = 1.0
    c["c_ohs"] = ohs
    bm8 = np.zeros((8, 8, 64), np.float32)
    for h in range(8):
        bm8[h, h, :] = 1.0
    c["c_bm8"] = bm8.reshape(8, 512)
    gm = np.zeros((4, 8, 8), np.float32)
    for a in range(4):
        gm[a, :, a + 4:] = -1e30
    c["c_nio"] = np.tile(gm.reshape(1, 256), (128, 1))
    return c


class K:
    pass


def build(nc, es, stage=99, dbg=False, groups=None):
    groups = GROUPS if groups is None else groups
    P = Prog(nc, es)
    k = K()
    k.P = P

    def DI(name, shape, dt=F32):
        return nc.dram_tensor(name, list(shape), dt, kind="ExternalInput").ap()

    def DO(name, shape, dt=F32):
        return nc.dram_tensor(name, list(shape), dt, kind="ExternalOutput").ap()

    def DS(name, shape, dt=F32):
        return Buf(nc.dram_tensor(name, list(shape), dt, kind="Internal").ap(), name)

    x_d = DI("x", [NT_ALL * 128, D_MODEL])
    p_d = DI("p", [NT_ALL * 128, PLE_DIM])
    ck_d = DI("cache_k", [2560 * 128, 512])
    cv_d = DI("cache_v", [2560 * 128, 512])
    pt_d = DI("page_table", [4, N_PAGES], I32)
    swkv_d = DI("state_wkv", [4, 8, 64, 64])
    ssh_d = DI("state_shift", [4, RW_COLS])
    n1_d = DI("norm1_g", [D_MODEL])
    win_d = DI("w_in", [D_MODEL, IN_COLS])
    rb_d = DI("rel_bias", [32, 8])
    mu_d = DI("shift_mu", [RW_COLS])
    w0_d = DI("w0", [W_R])
    w2_d = DI("w2", [64, W_R])
    a0_d = DI("a0", [W_R])
    a2_d = DI("a2", [64, W_R])
    g2_d = DI("g2", [160, W_R])
    kk_d = DI("k_k", [W_R])
    ka_d = DI("k_a", [W_R])
    rk_d = DI("r_k", [W_R])
    lg_d = DI("lnx_g", [W_R])
    lb_d = DI("lnx_b", [W_R])
    wout_d = DI("w_out", [1024, D_MODEL])
    n2_d = DI("norm2_g", [D_MODEL])
    wrg_d = DI("w_rg", [D_MODEL, 4])
    brg_d = DI("b_rg", [4])
    wre_d = DI("w_re", [D_MODEL, 16])
    bre_d = DI("b_re", [16])
    weg_d = DI("w_eg", [N_EXP, D_MODEL, D_EXP])
    weu_d = DI("w_eu", [N_EXP, D_MODEL, D_EXP])
    wed_d = DI("w_ed", [N_EXP, D_EXP, D_MODEL])
    wple_d = DI("w_ple", [PLE_DIM, D_MODEL])
    wpleg_d = DI("w_pleg", [D_MODEL, D_MODEL])
    fg_d = DI("final_g", [D_MODEL])
    c_ident = DI("c_ident", [128, 128])
    c_masks = DI("c_masks", [128, 3, 128])
    c_bones = DI("c_bones", [128, 128])
    c_ind = DI("c_ind", [128, 2])
    c_oh = DI("c_oh", [32, 384])
    c_negf = DI("c_negf", [128, 384])
    c_ohs = DI("c_ohs", [32, 128])
    c_bm8 = DI("c_bm8", [8, 512])
    c_nio = DI("c_nio", [128, 256])

    y_o = DO("y", [NT_ALL * 128, D_MODEL])
    kp_o = DO("k_new", [NT_ALL * 128, 512])
    vp_o = DO("v_new", [NT_ALL * 128, 512])
    wkvp_o = DO("wkv_p", [8, 64, 64])
    wkvs_o = DO("wkv_s", [4, 8, 64, 64])
    shp_o = DO("shift_p", [RW_COLS])
    shs_o = DO("shift_s", [4, RW_COLS])

    x1_s = DS("x1_scr", [NT_ALL * 128, D_MODEL])
    bias_s = DS("bias_scr", [8, 128 * 384])
    sq_s = DS("sq_scr", [2, 3, 128, 512])
    smix_s = DS("smix_scr", [2, 128, 4, 128], BF16)

    banks = [P.ps("pb%d" % i, [128, 512], F32) for i in range(8)]
    k.bi = 0

    k.reserved = set()

    def bank(reserve=False):
        while True:
            i = k.bi % 8
            k.bi += 1
            if i not in k.reserved:
                break
        if reserve:
            k.reserved.add(i)
        return banks[i]

    def release(b):
        k.reserved.discard(banks.index(b))

    def mm(bk, out, lhsT, rhs, reads, start=True, stop=True):
        P.op("pe", lambda e: e.matmul(out, lhsT, rhs, start=start, stop=stop), reads=reads, writes=[bk])

    def tr(bk, out, in_, ident, reads):
        P.op("pe", lambda e: e.transpose(out, in_, ident), reads=reads, writes=[bk])

    def act(out, in_, func, reads, writes, bias=None, scale=None, accum=None, eng="act"):
        kw = {}
        if bias is not None:
            kw["bias"] = bias
        if scale is not None:
            kw["scale"] = scale
        if accum is not None:
            kw["accum_out"] = accum
        P.op("act", lambda e: e.activation(out=out, in_=in_, func=func, **kw), reads=reads, writes=writes)

    def tt(eng, out, in0, in1, op, reads, writes):
        P.op(eng, lambda e: e.tensor_tensor(out=out, in0=in0, in1=in1, op=op), reads=reads, writes=writes)

    def ts(eng, out, in0, s1, s2, op0, op1, reads, writes):
        if op1 is None:
            P.op(eng, lambda e: e.tensor_scalar(out=out, in0=in0, scalar1=s1, scalar2=None, op0=op0),
                 reads=reads, writes=writes)
        else:
            P.op(eng, lambda e: e.tensor_scalar(out=out, in0=in0, scalar1=s1, scalar2=s2, op0=op0, op1=op1),
                 reads=reads, writes=writes)

    def stt(out, in0, scalar, in1, op0, op1, reads, writes):
        P.op("dve", lambda e: e.scalar_tensor_tensor(out=out, in0=in0, scalar=scalar, in1=in1, op0=op0, op1=op1),
             reads=reads, writes=writes)

    def cp(eng, out, in_, reads, writes):
        if eng == "act":
            P.op("act", lambda e: e.copy(out=out, in_=in_), reads=reads, writes=writes)
        else:
            P.op(eng, lambda e: e.tensor_copy(out=out, in_=in_), reads=reads, writes=writes)

    def red(out, in_, op, reads, writes, axis=AX.X):
        P.op("dve", lambda e: e.tensor_reduce(out=out, in_=in_, axis=axis, op=op), reads=reads, writes=writes)

    def ld(out_buf, out, in_, eng="sp", reads=(), nc_ok=False):
        if nc_ok:
            P.dma(eng, lambda e: e.dma_start(out=out, in_=in_, allow_slow_non_contiguous=True), reads=list(reads), writes=[out_buf])
        else:
            P.dma(eng, lambda e: e.dma_start(out=out, in_=in_), reads=list(reads), writes=[out_buf])

    def st(out, in_, reads, writes=(), eng="sp", nc_ok=False):
        if nc_ok:
            P.dma(eng, lambda e: e.dma_start(out=out, in_=in_, allow_slow_non_contiguous=True), reads=list(reads), writes=list(writes))
        else:
            P.dma(eng, lambda e: e.dma_start(out=out, in_=in_), reads=list(reads), writes=list(writes))

    def memset(eng, buf, ap, val):
        P.op(eng, lambda e: e.memset(ap, val), reads=[], writes=[buf])

    def bfv(b):
        return b.ap.bitcast(BF16)

    ident_f = P.sb("ident_f", [128, 128], F32)
    ident_b = P.sb("ident_b", [128, 128], BF16)
    masks = P.sb("masks", [128, 3, 128], F32)
    bones_f = P.sb("bones_f", [128, 128], F32)
    bones_b = P.sb("bones_b", [128, 128], BF16)
    ind_f = P.sb("ind_f", [128, 2], F32)
    ind_b = P.sb("ind_b", [128, 2], BF16)
    ld(ident_f, ident_f.ap, c_ident)
    ld(masks, masks.ap, c_masks)
    ld(bones_f, bones_f.ap, c_bones)
    ld(ind_f, ind_f.ap, c_ind)
    nio = P.sb("nio", [128, 4, 64], F32)
    ld(nio, nio.ap.rearrange("p a n -> p (a n)"), c_nio)
    cp("dve", ident_b.ap, ident_f.ap, [ident_f], [ident_b])
    cp("dve", bones_b.ap, bones_f.ap, [bones_f], [bones_b])
    cp("dve", ind_b.ap, ind_f.ap, [ind_f], [ind_b])

    g1c = P.sb("g1c", [128, 8], F32)
    g2c = P.sb("g2c", [128, 8], F32)
    ld(g1c, g1c.ap, n1_d.rearrange("(c p) -> p c", p=128), nc_ok=True)
    ld(g2c, g2c.ap, n2_d.rearrange("(c p) -> p c", p=128), nc_ok=True)
    muc = P.sb("muc", [128, 15], F32)
    memset("pool", muc, muc.ap, 0.0)
    ld(muc, muc.ap[:, 0:14], mu_d[0:1792].rearrange("(c p) -> p c", p=128), nc_ok=True)
    ld(muc, muc.ap[0:32, 14:15], mu_d[1792:1824].rearrange("(c p) -> p c", p=32), nc_ok=True)
    prm = {}
    for nm, d in (("w0", w0_d), ("a0", a0_d), ("kk", kk_d), ("ka", ka_d), ("rk", rk_d)):
        t = P.sb("prm_" + nm, [128, 4], F32)
        ld(t, t.ap, d.rearrange("(c p) -> p c", p=128), nc_ok=True)
        prm[nm] = t
    omka = P.sb("omka", [128, 4], F32)
    ts("dve", omka.ap, prm["ka"].ap, -1.0, 1.0, ALU.mult, ALU.add, [prm["ka"]], [omka])
    lgb = P.sb("lgb", [128, 512], F32)
    lbb = P.sb("lbb", [128, 512], F32)
    ld(lgb, lgb.ap, lg_d.partition_broadcast(128))
    ld(lbb, lbb.ap, lb_d.partition_broadcast(128))
    b31 = P.sb("b31", [128, 8], F32)
    ld(b31, b31.ap, rb_d[31, :].partition_broadcast(128))

    phS = ExitStack()
    P.es = phS
    wout_b = P.sb("wout_b", [128, 8, 1024], BF16)
    ph1 = ExitStack()
    P.es = ph1
    win_b = P.sb("win_b", [128, 8, IN_COLS], BF16)
    wa2_b = P.sb("wa2_b", [128, 512], BF16)
    g2a_b = P.sb("g2a_b", [128, 512], BF16)
    g2b_b = P.sb("g2b_b", [32, 512], BF16)
    ph0 = ExitStack()
    P.es = ph0
    stg = [P.sb("stg%d" % i, [128, 1680], F32) for i in range(2)]
    k.si = 0

    def load_cast(dst_buf, dst_ap, src_ap, shape_cols, eng="pool"):
        s = stg[k.si % 2]
        k.si += 1
        sv = s.ap[0:dst_ap.shape[0], 0:shape_cols]
        ld(s, sv, src_ap)
        if len(dst_ap.shape) == 3:
            sv = sv.rearrange("p (a b) -> p a b", a=dst_ap.shape[1])
        cp(eng, dst_ap, sv, [s], [dst_buf])

    for c in range(8):
        for hf in range(2):
            load_cast(win_b, win_b.ap[:, c, hf * 1680:(hf + 1) * 1680], win_d[c * 128:(c + 1) * 128, hf * 1680:(hf + 1) * 1680],
                      1680, eng=("pool" if hf else "dve"))
    for c in range(8):
        load_cast(wout_b, wout_b.ap[:, c, :], wout_d[c * 128:(c + 1) * 128, :], 1024)
    load_cast(wa2_b, wa2_b.ap[0:64, :], w2_d, 512)
    s = stg[k.si % 2]; k.si += 1
    ld(s, s.ap[64:128, 0:512], a2_d)
    cp("pool", wa2_b.ap[64:128, :], s.ap[64:128, 0:512], [s], [wa2_b])
    load_cast(g2a_b, g2a_b.ap, g2_d[0:128, :], 512)
    load_cast(g2b_b, g2b_b.ap, g2_d[128:160, :], 512)


    P.flush()
    ph0.close()
    P.es = ph1
    qT_g = P.sb("qT_g", [128, 4, 128], BF16)
    kT_all = P.sb("kT_all", [128, 4, 2048], BF16)
    qT_s = P.sb("qT_s", [128, 4, 128], BF16)
    kT_s = P.sb("kT_s", [128, 4, 128], BF16)
    kmT = P.sb("kmT", [128, 4, 8], F32)
    kmT_b = P.sb("kmT_b", [128, 4, 8], BF16)
    memset("pool", kmT, kmT.ap, 0.0)
    memset("pool", kmT_b, kmT_b.ap, 0.0)
    v_aug = P.sb("v_aug", [128, NTILE, 8, 66], BF16)
    memset("pool", v_aug, v_aug.ap, 1.0)
    zraw = P.sb("zraw", [128, 15, 129], F32)
    memset("pool", zraw, zraw.ap, 0.0)
    carry = P.sb("carry", [128, 15], F32)
    hT = P.sb("hT", [128, 8, 128], BF16)
    xt = [P.sb("xt%d" % i, [128, 1024], F32) for i in range(1)]
    k.xi = 0
    xs_b = P.sb("xs_b", [128, 1024], BF16)
    sqj = P.sb("sqj", [128, 1024], BF16)
    nst = P.sb("nst", [128, 8], F32)
    kvst = [P.sb("kvst%d" % i, [128, 2, 512], F32) for i in range(1)]
    k.kvi = 0

    def rms_to_T(src_ap, src_buf, gcol, dstT, dst_cols, stat):
        act(sqj.ap, src_ap, AF.Square, [src_buf], [sqj, stat], accum=stat.ap[:, 0:1])
        ts("dve", stat.ap[:, 1:2], stat.ap[:, 0:1], 1.0 / 1024, 1e-6, ALU.mult, ALU.add, [stat], [stat])
        act(stat.ap[:, 2:3], stat.ap[:, 1:2], AF.Sqrt, [stat], [stat])
        P.op("dve", lambda e: e.reciprocal(out=stat.ap[:, 3:4], in_=stat.ap[:, 2:3]), reads=[stat], writes=[stat])
        ts("dve", xs_b.ap, src_ap, stat.ap[:, 3:4], None, ALU.mult, None, [src_buf, stat], [xs_b])
        bk = bank()
        bv = bfv(bk)
        for c in range(8):
            tr(bk, bv[:, c * 128:(c + 1) * 128], xs_b.ap[:, c * 128:(c + 1) * 128], ident_b.ap, [xs_b, ident_b])
        tt("dve", dstT.ap[:, :, dst_cols], bv[:, 0:1024].rearrange("p (c t) -> p c t", c=8),
           gcol.ap.unsqueeze(2).to_broadcast([128, 8, 128]), ALU.mult, [bk, gcol], [dstT])

    def phase1_proj(gi, t0, nt):
        ntok = nt * 128
        tok0 = t0 * 128
        sample = (t0 >= 16)
        for i in range(nt):
            tile = t0 + i
            xb = xt[0]
            k.xi += 1
            ld(xb, xb.ap, x_d[tile * 128:(tile + 1) * 128, :])
            rms_to_T(xb.ap, xb, g1c, hT, slice(i * 128, (i + 1) * 128), nst)
        import os
        SKIP = os.environ.get('DBG_SKIP', '')
        for hp in range(0 if 'qk' in SKIP else 4):
            for which in range(2):
                col0 = which * 512 + hp * 128
                bk = bank()
                for c in range(8):
                    mm(bk, bk.ap[:, 0:ntok], win_b.ap[:, c, col0:col0 + 128], hT.ap[:, c, 0:ntok], [win_b, hT],
                       start=(c == 0), stop=(c == 7))
                if which == 0:
                    dst = qT_s if sample else qT_g
                    dcols = slice(0, ntok)
                else:
                    dst = kT_s if sample else kT_all
                    dcols = slice(0, 128) if sample else slice(tok0, tok0 + ntok)
                if which == 1 and not sample:
                    blk = t0 // 2
                    act(dst.ap[:, hp, dcols], bk.ap[:, 0:ntok], AF.Identity, [bk], [dst, nst], accum=nst.ap[:, 4 + hp:5 + hp])
                    if t0 % 2 == 0:
                        cp("dve", kmT.ap[:, hp, blk:blk + 1], nst.ap[:, 4 + hp:5 + hp], [nst], [kmT])
                    else:
                        tt("dve", kmT.ap[:, hp, blk:blk + 1], kmT.ap[:, hp, blk:blk + 1], nst.ap[:, 4 + hp:5 + hp], ALU.add, [kmT, nst], [kmT])
                else:
                    cp("act", dst.ap[:, hp, dcols], bk.ap[:, 0:ntok], [bk], [dst])
        if not sample and t0 % 2 == 1 and 'red' not in SKIP:
            blk = t0 // 2
            ts("dve", kmT_b.ap[:, :, blk:blk + 1], kmT.ap[:, :, blk:blk + 1], 1.0 / 256, None, ALU.mult, None, [kmT], [kmT_b])
        if gi > 0 and not sample:
            cp("pool", zraw.ap[:, :, 0], carry.ap, [carry], [zraw])
        for ct in range(0 if 'zz' in SKIP else 15):
            m = 128 if ct < 14 else 32
            col0 = 1536 + ct * 128
            bk = bank()
            for c in range(8):
                mm(bk, bk.ap[0:m, 0:ntok], win_b.ap[:, c, col0:col0 + m], hT.ap[:, c, 0:ntok], [win_b, hT],
                   start=(c == 0), stop=(c == 7))
            cp("act" if ct % 2 else "dve", zraw.ap[0:m, ct, 1:1 + ntok], bk.ap[0:m, 0:ntok], [bk], [zraw])
        import os
        SKIP = os.environ.get('DBG_SKIP', '')
        if 'shift' in SKIP:
            pass
        elif sample:
            for b, r in SMAP[t0]:
                st(shs_o[b, 0:1792].rearrange("(c p) -> p c", p=128), zraw.ap[:, 0:14, 1 + r], [zraw], nc_ok=True)
                st(shs_o[b, 1792:1824].rearrange("(c p) -> p c", p=32), zraw.ap[0:32, 14:15, 1 + r], [zraw], nc_ok=True)
            for b, r in SMAP[t0]:
                ld(zraw, zraw.ap[:, 0:14, r], ssh_d[b, 0:1792].rearrange("(c p) -> p c", p=128), nc_ok=True)
                ld(zraw, zraw.ap[0:32, 14:15, r], ssh_d[b, 1792:1824].rearrange("(c p) -> p c", p=32), nc_ok=True)
        else:
            cp("pool", carry.ap, zraw.ap[:, :, ntok], [zraw], [carry])
            if t0 + nt == 16:
                st(shp_o[0:1792].rearrange("(c p) -> p c", p=128), carry.ap[:, 0:14], [carry], nc_ok=True)
                st(shp_o[1792:1824].rearrange("(c p) -> p c", p=32), carry.ap[0:32, 14:15], [carry], nc_ok=True)
        for i in range(0 if 'kv' in SKIP else nt):
            tile = t0 + i
            bkk = bank()
            bkv = bank()
            for c in range(8):
                mm(bkk, bkk.ap[:, 0:512], hT.ap[:, c, i * 128:(i + 1) * 128], win_b.ap[:, c, 512:1024], [win_b, hT],
                   start=(c == 0), stop=(c == 7))
            for c in range(8):
                mm(bkv, bkv.ap[:, 0:512], hT.ap[:, c, i * 128:(i + 1) * 128], win_b.ap[:, c, 1024:1536], [win_b, hT],
                   start=(c == 0), stop=(c == 7))
            s_ = kvst[0]
            k.kvi += 1
            cp("act", s_.ap[:, 0, :], bkk.ap[:, 0:512], [bkk], [s_])
            cp("dve", s_.ap[:, 1, :], bkv.ap[:, 0:512], [bkv], [s_])
            st(kp_o[tile * 128:(tile + 1) * 128, :], s_.ap[:, 0, :], [s_])
            st(vp_o[tile * 128:(tile + 1) * 128, :], s_.ap[:, 1, :], [s_])
            if sample:
                si = tile - 16
                st(sq_s.ap[si, 1], s_.ap[:, 0, :], [s_], [sq_s])
                st(sq_s.ap[si, 2], s_.ap[:, 1, :], [s_], [sq_s])
                bkq = bank()
                for c in range(8):
                    mm(bkq, bkq.ap[:, 0:512], hT.ap[:, c, i * 128:(i + 1) * 128], win_b.ap[:, c, 0:512], [win_b, hT],
                       start=(c == 0), stop=(c == 7))
                cp("act", s_.ap[:, 0, :], bkq.ap[:, 0:512], [bkq], [s_])
                st(sq_s.ap[si, 0], s_.ap[:, 0, :], [s_], [sq_s])
            if not sample and 'vaug' not in SKIP:
                cp("dve", v_aug.ap[:, tile, :, 0:64], bkv.ap[:, 0:512].rearrange("p (h d) -> p h d", h=8), [bkv], [v_aug])


    def T(name, shape, dt):
        return P.sb(name, shape, dt)
    zs = T("zs", [128, 15, 128], F32)
    dtmp = T("dtmp", [128, 128], F32)
    lor_b = T("lor_b", [128, 128], BF16)
    sxg = T("sxg", [128, 128], BF16)
    sxg2 = T("sxg2", [32, 128], BF16)
    sigw = T("sigw", [128, 4, 128], F32)
    Aa = T("Aa", [128, 4, 128], F32)
    cl = T("cl", [128, 4, 128], F32)
    kkr = T("kkr", [128, 4, 128], F32)
    khh = T("khh", [128, 4, 128], F32)
    bb = T("bb", [128, 4, 128], F32)
    E1 = T("E1", [128, 4, 128], F32)
    E2 = T("E2", [128, 4, 128], F32)
    tm1 = T("tm1", [128, 4, 128], F32)
    gC = T("gC", [128, 4], F32)
    ones_f = T("ones_f", [128, 128], F32)
    memset("pool", ones_f, ones_f.ap, 1.0)
    sq_b = T("sq_b", [128, 4, 128], BF16)
    bt_b = T("bt_b", [128, 4, 128], BF16)
    ktl_b = T("ktl_b", [128, 4, 128], BF16)
    kr_b = T("kr_b", [128, 4, 2, 128], BF16)
    bh_b = T("bh_b", [128, 4, 128], BF16)
    kh_b = T("kh_b", [128, 4, 128], BF16)
    prod_b = T("prod_b", [128, 4, 128], BF16)
    vf_b = T("vf_b", [128, 4, 128], BF16)
    V_tm = T("V_tm", [128, 512], BF16)
    Bh_tm = T("Bh_tm", [128, 512], BF16)
    Kh_tm = T("Kh_tm", [128, 512], BF16)
    rkr = T("rkr", [128, 8], F32)
    Mb = [T("Mb%d" % i, [128, 2, 128], BF16) for i in range(2)]
    Mk = [T("Mk%d" % i, [128, 2, 128], BF16) for i in range(2)]
    Lt = [T("Lt%d" % i, [128, 128], BF16) for i in range(3)]
    Mt = [T("Mt%d" % i, [128, 128], BF16) for i in range(3)]
    Qt = [T("Qt%d" % i, [128, 128], BF16) for i in range(3)]
    W_b = [T("W_b%d" % i, [128, 64], BF16) for i in range(2)]
    U_b = T("U_b", [128, 8, 64], BF16)
    ST_f = [T("ST_f%d" % i, [128, 64], F32) for i in range(4)]
    ST_b = [T("ST_b%d" % i, [128, 64], BF16) for i in range(4)]
    ysb = T("ysb", [128, 8, 64], F32)
    yc = T("yc", [128, 8, 64], F32)
    ysq = T("ysq", [128, 8, 64], F32)
    gst = T("gst", [128, 4, 8], F32)
    rw_b = T("rw_b", [128, 512], BF16)
    mixT = T("mixT", [128, 8, 128], BF16)
    k.rot = 0

    def bc3(ap2, n):
        return ap2.unsqueeze(2).to_broadcast([ap2.shape[0], ap2.shape[1], n])

    def rwkv_prep(c0, C, single=False):
        zc = slice(1 + c0, 1 + c0 + C)
        zp = slice(c0, c0 + C)
        for ct in range(15):
            m = 128 if ct < 14 else 32
            tt("pool", dtmp.ap[0:m, 0:C], zraw.ap[0:m, ct, zp], zraw.ap[0:m, ct, zc], ALU.subtract, [zraw], [dtmp])
            stt(zs.ap[0:m, ct, 0:C], dtmp.ap[0:m, 0:C], muc.ap[0:m, ct:ct + 1], zraw.ap[0:m, ct, zc], ALU.mult, ALU.add,
                [dtmp, muc, zraw], [zs])
        R = zs.ap[:, 0:4, 0:C]
        Kf = zs.ap[:, 4:8, 0:C]
        Vf = zs.ap[:, 8:12, 0:C]
        act(lor_b.ap[0:64, 0:C], zs.ap[0:64, 12, 0:C], AF.Tanh, [zs], [lor_b])
        cp("pool", lor_b.ap[64:128, 0:C], zs.ap[64:128, 12, 0:C], [zs], [lor_b])
        act(sxg.ap[:, 0:C], zs.ap[:, 13, 0:C], AF.Sigmoid, [zs], [sxg])
        act(sxg2.ap[:, 0:C], zs.ap[0:32, 14, 0:C], AF.Sigmoid, [zs], [sxg2])
        bk = bank()
        bk2 = bank()
        for hp in range(4):
            mm(bk, bk.ap[:, hp * 128:hp * 128 + C], wa2_b.ap[0:64, hp * 128:(hp + 1) * 128], lor_b.ap[0:64, 0:C], [wa2_b, lor_b])
            mm(bk2, bk2.ap[:, hp * 128:hp * 128 + C], wa2_b.ap[64:128, hp * 128:(hp + 1) * 128], lor_b.ap[64:128, 0:C], [wa2_b, lor_b])
        for hp in range(4):
            act(sigw.ap[:, hp, 0:C], bk.ap[:, hp * 128:hp * 128 + C], AF.Sigmoid, [bk, prm["w0"]], [sigw], bias=prm["w0"].ap[:, hp:hp + 1])
            act(Aa.ap[:, hp, 0:C], bk2.ap[:, hp * 128:hp * 128 + C], AF.Sigmoid, [bk2, prm["a0"]], [Aa], bias=prm["a0"].ap[:, hp:hp + 1])
        ts("dve", sigw.ap[:, :, 0:C], sigw.ap[:, :, 0:C], -0.6065306597126334, None, ALU.mult, None, [sigw], [sigw])
        if single:
            cp("dve", cl.ap[:, :, 0:C], sigw.ap[:, :, 0:C], [sigw], [cl])
        for hp in range(0 if single else 4):
            P.op("dve", lambda e, hp=hp: e.tensor_tensor_scan(out=cl.ap[:, hp, 0:C], data0=ones_f.ap[:, 0:C], data1=sigw.ap[:, hp, 0:C],
                                                               initial=0.0, op0=ALU.mult, op1=ALU.add), reads=[ones_f, sigw], writes=[cl])
        tt("dve", kkr.ap[:, :, 0:C], Kf, bc3(prm["kk"].ap, C), ALU.mult, [zs, prm["kk"]], [kkr])
        tt("pool", sq_b.ap[:, :, 0:C], kkr.ap[:, :, 0:C], kkr.ap[:, :, 0:C], ALU.mult, [kkr], [sq_b])
        bk = bank()
        for hp in range(4):
            mm(bk, bk.ap[:, hp * 128:hp * 128 + C], bones_b.ap, sq_b.ap[:, hp, 0:C], [bones_b, sq_b])
        bkv = bk.ap[:, 0:512].rearrange("p (a b) -> p a b", a=4)[:, :, 0:C]
        ts("dve", tm1.ap[:, :, 0:C], bkv, 1e-24, None, ALU.max, None, [bk], [tm1])
        act(tm1.ap[:, :, 0:C], tm1.ap[:, :, 0:C], AF.Sqrt, [tm1], [tm1])
        P.op("dve", lambda e: e.reciprocal(out=tm1.ap[:, :, 0:C], in_=tm1.ap[:, :, 0:C]), reads=[tm1], writes=[tm1])
        tt("dve", kkr.ap[:, :, 0:C], kkr.ap[:, :, 0:C], tm1.ap[:, :, 0:C], ALU.mult, [kkr, tm1], [kkr])
        tt("pool", khh.ap[:, :, 0:C], Aa.ap[:, :, 0:C], bc3(prm["ka"].ap, C), ALU.mult, [Aa, prm["ka"]], [khh])
        tt("pool", khh.ap[:, :, 0:C], khh.ap[:, :, 0:C], bc3(omka.ap, C), ALU.add, [khh, omka], [khh])
        tt("pool", khh.ap[:, :, 0:C], khh.ap[:, :, 0:C], Kf, ALU.mult, [khh, zs], [khh])
        tt("dve", bb.ap[:, :, 0:C], kkr.ap[:, :, 0:C], Aa.ap[:, :, 0:C], ALU.mult, [kkr, Aa], [bb])
        act(E1.ap[:, :, 0:C], cl.ap[:, :, 0:C], AF.Exp, [cl], [E1])
        tt("dve", kr_b.ap[:, :, 1, 0:C], R, E1.ap[:, :, 0:C], ALU.mult, [zs, E1], [kr_b])
        tt("pool", tm1.ap[:, :, 0:C], cl.ap[:, :, 0:C], sigw.ap[:, :, 0:C], ALU.subtract, [cl, sigw], [tm1])
        act(E2.ap[:, :, 0:C], tm1.ap[:, :, 0:C], AF.Exp, [tm1], [E2])
        tt("dve", kr_b.ap[:, :, 0, 0:C], kkr.ap[:, :, 0:C], E2.ap[:, :, 0:C], ALU.mult, [kkr, E2], [kr_b])
        act(E1.ap[:, :, 0:C], cl.ap[:, :, 0:C], AF.Exp, [cl], [E1], scale=-1.0)
        tt("dve", bt_b.ap[:, :, 0:C], bb.ap[:, :, 0:C], E1.ap[:, :, 0:C], ALU.mult, [bb, E1], [bt_b])
        tt("pool", ktl_b.ap[:, :, 0:C], khh.ap[:, :, 0:C], E1.ap[:, :, 0:C], ALU.mult, [khh, E1], [ktl_b])
        if single:
            memset("pool", E2, E2.ap, 1.0)
        for hp in range(0 if single else 4):
            act(E2.ap[:, hp, 0:C], cl.ap[:, hp, 0:C], AF.Exp, [cl], [E2], scale=-1.0, bias=cl.ap[:, hp, C - 1:C])
        tt("dve", bh_b.ap[:, :, 0:C], bb.ap[:, :, 0:C], E2.ap[:, :, 0:C], ALU.mult, [bb, E2], [bh_b])
        tt("pool", kh_b.ap[:, :, 0:C], khh.ap[:, :, 0:C], E2.ap[:, :, 0:C], ALU.mult, [khh, E2], [kh_b])
        act(gC.ap, cl.ap[:, :, C - 1], AF.Exp, [cl], [gC])
        tt("pool", tm1.ap[:, :, 0:C], R, khh.ap[:, :, 0:C], ALU.mult, [zs, khh], [tm1])
        tt("pool", prod_b.ap[:, :, 0:C], tm1.ap[:, :, 0:C], bc3(prm["rk"].ap, C), ALU.mult, [tm1, prm["rk"]], [prod_b])
        cp("pool", vf_b.ap[:, :, 0:C], Vf, [zs], [vf_b])

    def rwkv_tm(C):
        bk = bank()
        for hp in range(4):
            mm(bk, bk.ap[0:C, 2 * hp:2 * hp + 2], prod_b.ap[:, hp, 0:C], ind_b.ap, [prod_b, ind_b])
        cp("dve", rkr.ap[0:C, :], bk.ap[0:C, 0:8], [bk], [rkr])
        for src_, dst_ in ((vf_b, V_tm), (bh_b, Bh_tm), (kh_b, Kh_tm)):
            bk = bank()
            bv = bfv(bk)
            for hp in range(4):
                tr(bk, bv[0:C, hp * 128:(hp + 1) * 128], src_.ap[:, hp, 0:C], ident_b.ap, [src_, ident_b])
            cp("act", dst_.ap[0:C, :], bv[0:C, 0:512], [bk], [dst_])

    def rwkv_chunk(C, r0=0, c0=0, ybank=None):
        rs = slice(r0, r0 + C)
        cs = slice(c0, c0 + C)
        yb = bank(reserve=True) if ybank is None else ybank
        nlev = 0
        while (1 << nlev) < C:
            nlev += 1
        for hp in range(4):
            for h2 in range(2):
                h = 2 * hp + h2
                ps_ = slice(h2 * 64, h2 * 64 + 64)
                mb = Mb[h % 2]
                mk = Mk[h % 2]
                b1 = bank()
                mm(b1, b1.ap[rs, 0:2 * 128].rearrange("p (a b) -> p a b", a=2)[:, :, 0:C], bt_b.ap[ps_, hp, cs], kr_b.ap[ps_, hp, :, cs], [bt_b, kr_b])
                tt("dve", mb.ap[rs, :, 0:C], b1.ap[rs, 0:256].rearrange("p (a b) -> p a b", a=2)[:, :, 0:C], masks.ap[0:C, 0:2, 0:C],
                   ALU.mult, [b1, masks], [mb])
                b2 = bank()
                mm(b2, b2.ap[rs, 0:256].rearrange("p (a b) -> p a b", a=2)[:, :, 0:C], ktl_b.ap[ps_, hp, cs], kr_b.ap[ps_, hp, :, cs], [ktl_b, kr_b])
                tt("dve", mk.ap[rs, :, 0:C], b2.ap[rs, 0:256].rearrange("p (a b) -> p a b", a=2)[:, :, 0:C], masks.ap[0:C, 0:2, 0:C],
                   ALU.mult, [b2, masks], [mk])
                qcur = Qt[k.rot % 3]
                if C > 1:
                    b3 = bank()
                    mm(b3, b3.ap[rs, 0:C], kr_b.ap[ps_, hp, 0, cs], bt_b.ap[ps_, hp, cs], [kr_b, bt_b])
                    lcur = Lt[k.rot % 3]
                    mcur = Mt[k.rot % 3]
                    tt("dve", lcur.ap[rs, 0:C], b3.ap[rs, 0:C], masks.ap[0:C, 2, 0:C], ALU.mult, [b3, masks], [lcur])
                    cp("act", mcur.ap[rs, 0:C], mb.ap[rs, 0, 0:C], [mb], [mcur])
                    tt("dve", qcur.ap[rs, 0:C], ident_f.ap[0:C, 0:C], mb.ap[rs, 0, 0:C], ALU.subtract, [ident_f, mb], [qcur])
                    for lev in range(1, nlev + 1):
                        k.rot += 1
                        lnew = Lt[k.rot % 3]
                        mnew = Mt[k.rot % 3]
                        qnew = Qt[k.rot % 3]
                        bl = bank()
                        mm(bl, bl.ap[rs, 0:C], mcur.ap[rs, 0:C], lcur.ap[rs, 0:C], [mcur, lcur])
                        cp("act", lnew.ap[rs, 0:C], bl.ap[rs, 0:C], [bl], [lnew])
                        if lev < nlev:
                            bm = bank()
                            mm(bm, bm.ap[rs, 0:C], lcur.ap[rs, 0:C], mcur.ap[rs, 0:C], [mcur, lcur])
                            cp("act", mnew.ap[rs, 0:C], bm.ap[rs, 0:C], [bm], [mnew])
                        bq = bank()
                        mm(bq, bq.ap[rs, 0:C], lnew.ap[rs, 0:C], qcur.ap[rs, 0:C], [lnew, qcur])
                        tt("dve", qnew.ap[rs, 0:C], bq.ap[rs, 0:C], qcur.ap[rs, 0:C], ALU.add, [bq, qcur], [qnew])
                        lcur, mcur, qcur = lnew, mnew, qnew
                    k.rot += 1
                else:
                    cp("dve", qcur.ap[rs, 0:C], ident_f.ap[0:1, 0:1], [ident_f], [qcur])
                    k.rot += 1
                wb = W_b[h % 2]
                bw = bank()
                mm(bw, bw.ap[rs, 0:64], kr_b.ap[ps_, hp, 0, cs], ST_b[hp].ap[ps_, :], [kr_b, ST_b[hp]], start=True, stop=False)
                mm(bw, bw.ap[rs, 0:64], mk.ap[rs, 0, 0:C], V_tm.ap[rs, h * 64:(h + 1) * 64], [mk, V_tm], start=False, stop=True)
                cp("act", wb.ap[rs, :], bw.ap[rs, 0:64], [bw], [wb])
                bu = bank()
                mm(bu, bu.ap[rs, 0:64], qcur.ap[rs, 0:C], wb.ap[rs, :], [qcur, wb])
                ts("dve", U_b.ap[rs, h, :], bu.ap[rs, 0:64], -1.0, None, ALU.mult, None, [bu], [U_b])
                yo = yb.ap[rs, h * 64:(h + 1) * 64]
                mm(yb, yo, kr_b.ap[ps_, hp, 1, cs], ST_b[hp].ap[ps_, :], [kr_b, ST_b[hp]], start=True, stop=False)
                mm(yb, yo, mb.ap[rs, 1, 0:C], U_b.ap[rs, h, :], [mb, U_b], start=False, stop=False)
                mm(yb, yo, mk.ap[rs, 1, 0:C], V_tm.ap[rs, h * 64:(h + 1) * 64], [mk, V_tm], start=False, stop=True)
            bs = bank()
            mm(bs, bs.ap[:, 0:128], Bh_tm.ap[rs, hp * 128:(hp + 1) * 128], U_b.ap[rs, 2 * hp:2 * hp + 2, :], [Bh_tm, U_b],
               start=True, stop=False)
            mm(bs, bs.ap[:, 0:128], Kh_tm.ap[rs, hp * 128:(hp + 1) * 128], V_tm.ap[rs, hp * 128:(hp + 1) * 128], [Kh_tm, V_tm],
               start=False, stop=True)
            for h2 in range(2):
                ps_ = slice(h2 * 64, h2 * 64 + 64)
                stt(ST_f[hp].ap[ps_, :], ST_f[hp].ap[ps_, :], gC.ap[ps_, hp:hp + 1], bs.ap[ps_, h2 * 64:(h2 + 1) * 64],
                    ALU.mult, ALU.add, [ST_f[hp], gC, bs], [ST_f[hp]])
            cp("pool", ST_b[hp].ap, ST_f[hp].ap, [ST_f[hp]], [ST_b[hp]])
        return yb

    def rwkv_post(yb, C, r0, dst_cols):
        rs = slice(r0, r0 + C)
        y3 = yb.ap[rs, 0:512].rearrange("p (h d) -> p h d", h=8)
        cp("act", ysb.ap[rs], y3, [yb], [ysb])
        red(gst.ap[rs, 0, :], ysb.ap[rs], ALU.add, [ysb], [gst])
        ts("dve", gst.ap[rs, 0, :], gst.ap[rs, 0, :], 1.0 / 64, None, ALU.mult, None, [gst], [gst])
        tt("dve", yc.ap[rs], ysb.ap[rs], bc3(gst.ap[rs, 0, :], 64), ALU.subtract, [ysb, gst], [yc])
        tt("pool", ysq.ap[rs], yc.ap[rs], yc.ap[rs], ALU.mult, [yc], [ysq])
        red(gst.ap[rs, 1, :], ysq.ap[rs], ALU.add, [ysq], [gst])
        ts("dve", gst.ap[rs, 1, :], gst.ap[rs, 1, :], 1.0 / 64, 64e-5, ALU.mult, ALU.add, [gst], [gst])
        act(gst.ap[rs, 2, :], gst.ap[rs, 1, :], AF.Sqrt, [gst], [gst])
        P.op("dve", lambda e: e.reciprocal(out=gst.ap[rs, 3, :], in_=gst.ap[rs, 2, :]), reads=[gst], writes=[gst])
        tt("dve", yc.ap[rs], yc.ap[rs], bc3(gst.ap[rs, 3, :], 64), ALU.mult, [yc, gst], [yc])
        ycf = yc.ap[rs].rearrange("p h d -> p (h d)")
        tt("pool", ycf, ycf, lgb.ap[rs, :], ALU.mult, [yc, lgb], [yc])
        tt("pool", ycf, ycf, lbb.ap[rs, :], ALU.add, [yc, lbb], [yc])
        tt("dve", ysq.ap[rs], V_tm.ap[rs, :].rearrange("p (h d) -> p h d", h=8), bc3(rkr.ap[rs, :], 64), ALU.mult, [V_tm, rkr], [ysq])
        tt("dve", yc.ap[rs], yc.ap[rs], ysq.ap[rs], ALU.add, [yc, ysq], [yc])
        bg = bank()
        mm(bg, bg.ap[rs, 0:512], sxg.ap[:, dst_cols] if False else sxg.ap[:, r0:r0 + C], g2a_b.ap, [sxg, g2a_b], start=True, stop=False)
        mm(bg, bg.ap[rs, 0:512], sxg2.ap[:, r0:r0 + C], g2b_b.ap, [sxg2, g2b_b], start=False, stop=True)
        tt("dve", rw_b.ap[rs, :], ycf, bg.ap[rs, 0:512], ALU.mult, [yc, bg], [rw_b])

    def rw_to_T(dst_cols):
        bk = bank()
        bv = bfv(bk)
        for hp in range(4):
            tr(bk, bv[:, hp * 128:(hp + 1) * 128], rw_b.ap[:, hp * 128:(hp + 1) * 128], ident_b.ap, [rw_b, ident_b])
        cp("act", mixT.ap[:, 4:8, dst_cols], bv[:, 0:512].rearrange("p (c t) -> p c t", c=4), [bk], [mixT])

    def st_out(dst):
        for hp in range(4):
            bk = bank()
            tr(bk, bk.ap[0:64, 0:128], ST_f[hp].ap, ident_f.ap, [ST_f[hp], ident_f])
            cp("dve", ysb.ap[0:64, 2 * hp:2 * hp + 2, :], bk.ap[0:64, 0:128].rearrange("p (h j) -> p h j", h=2), [bk], [ysb])
        st(dst.rearrange("h i j -> i h j"), ysb.ap[0:64, :, :], [ysb])

    def phase1_rwkv(gi, t0, nt):
        sample = (t0 >= 16)
        if not sample:
            if gi == 0:
                for hp in range(4):
                    memset("pool", ST_f[hp], ST_f[hp].ap, 0.0)
                    memset("pool", ST_b[hp], ST_b[hp].ap, 0.0)
            for i in range(nt):
                rwkv_prep(i * 128, 128)
                rwkv_tm(128)
                yb = rwkv_chunk(128)
                rwkv_post(yb, 128, 0, None)
                release(yb)
                rw_to_T(slice(i * 128, (i + 1) * 128))
            if t0 + nt == 16:
                st_out(wkvp_o)
        else:
            rwkv_prep(0, 128, single=True)
            rwkv_tm(128)
            memset("pool", rw_b, rw_b.ap, 0.0)
            yb = bank(reserve=True)
            for b, r in SMAP[t0]:
                for hp in range(4):
                    ld(ysq, ysq.ap[0:64, 0:2, :], swkv_d[b, 2 * hp:2 * hp + 2].rearrange("h i j -> i h j"))
                    bk = bank()
                    tr(bk, bk.ap[:, 0:64], ysq.ap[0:64, 0:2, :].rearrange("p h j -> p (h j)"), ident_f.ap[0:64, 0:64], [ysq, ident_f])
                    cp("dve", ST_f[hp].ap, bk.ap[:, 0:64], [bk], [ST_f[hp]])
                    cp("pool", ST_b[hp].ap, ST_f[hp].ap, [ST_f[hp]], [ST_b[hp]])
                act(gC.ap, cl.ap[:, :, r], AF.Exp, [cl], [gC])
                rwkv_chunk(1, r0=r, c0=r, ybank=yb)
                st_out(wkvs_o[b])
            for b, r in SMAP[t0]:
                rwkv_post(yb, 1, r, None)
            release(yb)
            rw_to_T(slice(0, 128))


    Bd_b = T("Bd_b", [128, 8, 128], BF16)
    Bo_b = T("Bo_b", [128, 8, 128], BF16)
    E8 = T("E8", [8, 8, 128], BF16)
    g1 = T("g1", [128, 8, 8], F32)
    g2_ = T("g2_", [128, 8, 8], F32)
    eqt = T("eqt", [128, 8, 8], F32)
    mx = T("mx", [128, 8], F32)
    seln = T("seln", [128, 8, 8], BF16)
    selT = T("selT", [8, 8, 128], BF16)
    PT_b = [T("PT_b%d" % i, [128, 4, 128], BF16) for i in range(2)]
    att_b = T("att_b", [128, 8, 64], BF16)
    rcp = T("rcp", [128, 8], F32)
    k.pti = 0

    def attn_setup():
        ld(rb_sb, rb_sb.ap, rb_d)
        ld(oh_sb, oh_sb.ap, c_oh)
        ld(negf_sb, negf_sb.ap, c_negf)
        cp("dve", rbB.ap, bc3(rb_sb.ap, 128), [rb_sb], [rbB])
        cp("dve", E8.ap, bc3(ident_f.ap[0:8, 0:8], 128), [ident_f], [E8])
        for h in range(8):
            bk = bank()
            mm(bk, bk.ap[:, 0:384], rbB.ap[:, h, :], oh_sb.ap, [rbB, oh_sb])
            ts("dve", Frow.ap, bk.ap[:, 0:384], b31.ap[:, h:h + 1], 8.0, ALU.subtract, ALU.mult, [bk, b31], [Frow])
            tt("dve", Frow.ap, Frow.ap, negf_sb.ap, ALU.add, [Frow, negf_sb], [Frow])
            st(bias_s.ap[h].rearrange("(p c) -> p c", c=384), Frow.ap, [Frow], [bias_s])
            for off, dst in ((127, Bd_b), (255, Bo_b)):
                src_ap = bass.AP(bias_s.ap.tensor, h * 128 * 384 + off, [[383, 128], [1, 128]])
                ld(Bst, Bst.ap, src_ap, reads=[bias_s])
                cp("dve", dst.ap[:, h, :], Bst.ap, [Bst], [dst])

    def attn_tile(qt, qc0, dst_cols):
        n_own = qt // 2
        import os
        need_sel = n_own >= 4 and 'nosel' not in os.environ.get('DBG_SKIP', '')
        qcs = slice(qc0, qc0 + 128)
        if need_sel and 'selB' not in os.environ.get('DBG_SKIP', ''):
            bk = bank()
            for h in range(8):
                ps_ = slice((h % 2) * 64, (h % 2) * 64 + 64)
                for n4 in range(2):
                    mm(bk, bk.ap[:, h * 8 + n4 * 4:h * 8 + n4 * 4 + 4], qT_g.ap[ps_, h // 2, qcs], kmT_b.ap[ps_, h // 2, n4 * 4:n4 * 4 + 4],
                       [qT_g, kmT_b])
            tt("dve", g1.ap.rearrange("p h n -> p (h n)"), bk.ap[:, 0:64], nio.ap[:, n_own - 4, :], ALU.add, [bk, nio], [g1])
            red(mx.ap, g1.ap, ALU.max, [g1], [mx])
            tt("dve", eqt.ap, g1.ap, bc3(mx.ap, 8), ALU.is_ge, [g1, mx], [eqt])
            stt(g2_.ap, eqt.ap, -3e30, g1.ap, ALU.mult, ALU.add, [eqt, g1], [g2_])
            red(mx.ap, g2_.ap, ALU.max, [g2_], [mx])
            tt("dve", eqt.ap, g2_.ap, bc3(mx.ap, 8), ALU.is_ge, [g2_, mx], [eqt])
            stt(g2_.ap, eqt.ap, -3e30, g2_.ap, ALU.mult, ALU.add, [eqt, g2_], [g2_])
            red(mx.ap, g2_.ap, ALU.max, [g2_], [mx])
            tt("dve", eqt.ap, g1.ap, bc3(mx.ap, 8), ALU.is_ge, [g1, mx], [eqt])
            ts("dve", seln.ap, eqt.ap, -NEGB, NEGB, ALU.mult, ALU.add, [eqt], [seln])
            bk = bank()
            bv = bfv(bk)
            for h in range(0 if 'selC' in os.environ.get('DBG_SKIP', '') else 8):
                tr(bk, bv[0:8, h * 128:(h + 1) * 128], seln.ap[:, h, :], ident_b.ap, [seln, ident_b])
            if 'selC' not in os.environ.get('DBG_SKIP', '') and 'selD' not in os.environ.get('DBG_SKIP', ''):
                cp("act", selT.ap, bv[0:8, 0:1024].rearrange("p (h q) -> p h q", h=8), [bk], [selT])
        OA = bank(reserve=True)
        OB = bank(reserve=True)
        for h in range(8):
            hp = h // 2
            ps_ = slice((h % 2) * 64, (h % 2) * 64 + 64)
            ob = OA if h < 4 else OB
            oreg = ob.ap[:, (h % 4) * 65:(h % 4) * 65 + 65]
            for kc0 in range(0, qt + 1, 4):
                kts = list(range(kc0, min(kc0 + 4, qt + 1)))
                sb_ = bank()
                for j, kt in enumerate(kts):
                    extra = []
                    if kt == qt:
                        extra.append((ident_b.ap, Bd_b.ap[:, h, :], [ident_b, Bd_b]))
                    elif kt == qt - 1:
                        extra.append((ident_b.ap, Bo_b.ap[:, h, :], [ident_b, Bo_b]))
                    if need_sel and kt // 2 < n_own and 'selA' not in os.environ.get('DBG_SKIP', ''):
                        extra.append((E8.ap[:, kt // 2, :], selT.ap[:, h, :], [E8, selT]))
                    so = sb_.ap[:, j * 128:(j + 1) * 128]
                    mm(sb_, so, kT_all.ap[ps_, hp, kt * 128:(kt + 1) * 128], qT_g.ap[ps_, hp, qcs], [kT_all, qT_g],
                       start=True, stop=(len(extra) == 0))
                    for ei, (l_, r_, rd_) in enumerate(extra):
                        mm(sb_, so, l_, r_, rd_, start=False, stop=(ei == len(extra) - 1))
                pt = PT_b[k.pti % 2]
                k.pti += 1
                nj = len(kts)
                act(pt.ap[:, 0:nj, :], sb_.ap[:, 0:nj * 128].rearrange("p (j q) -> p j q", j=nj), AF.Exp, [sb_, b31], [pt],
                    bias=b31.ap[:, h:h + 1], scale=0.125)
                for j, kt in enumerate(kts):
                    mm(ob, oreg, pt.ap[:, j, :], v_aug.ap[:, kt, h, 0:65], [pt, v_aug], start=(kt == 0), stop=(kt == qt))
        for ob, h0 in ((OA, 0), (OB, 4)):
            o3 = ob.ap[:, 0:260].rearrange("p (h d) -> p h d", h=4)
            P.op("dve", lambda e, o3=o3, h0=h0: e.reciprocal(out=rcp.ap[:, h0:h0 + 4], in_=o3[:, :, 64]), reads=[ob], writes=[rcp])
            tt("dve", att_b.ap[:, h0:h0 + 4, :], o3[:, :, 0:64], bc3(rcp.ap[:, h0:h0 + 4], 64), ALU.mult, [ob, rcp], [att_b])
        release(OA)
        release(OB)
        bk = bank()
        bv = bfv(bk)
        af = att_b.ap.rearrange("p h d -> p (h d)")
        for c in range(4):
            tr(bk, bv[:, c * 128:(c + 1) * 128], af[:, c * 128:(c + 1) * 128], ident_b.ap, [att_b, ident_b])
        cp("act", mixT.ap[:, 0:4, dst_cols], bv[:, 0:512].rearrange("p (c t) -> p c t", c=4), [bk], [mixT])

    x1t = Buf(zs.ap[:, 0:8, :].rearrange("p a b -> p (a b)"), "x1t")
    x1t_buf = zs

    def out_proj(tile, cols):
        xb = xt[0]
        ld(xb, xb.ap, x_d[tile * 128:(tile + 1) * 128, :])
        for hf in range(2):
            bk = bank()
            for c in range(8):
                mm(bk, bk.ap[:, 0:512], mixT.ap[:, c, cols], wout_b.ap[:, c, hf * 512:(hf + 1) * 512], [mixT, wout_b],
                   start=(c == 0), stop=(c == 7))
            tt("dve", x1t.ap[:, hf * 512:(hf + 1) * 512], bk.ap[:, 0:512], xb.ap[:, hf * 512:(hf + 1) * 512], ALU.add, [bk, xb], [x1t_buf])
        st(x1_s.ap[tile * 128:(tile + 1) * 128, :], x1t.ap, [x1t_buf], [x1_s])

    def phase1_rest(gi, t0, nt):
        sample = (t0 >= 16)
        for i in range(nt):
            cols = slice(i * 128, (i + 1) * 128)
            if not sample:
                import os
                if 'attn' not in os.environ.get('DBG_SKIP', ''):
                    attn_tile(t0 + i, i * 128, cols)
                out_proj(t0 + i, cols)
            else:
                st(smix_s.ap[t0 - 16], mixT.ap[:, 4:8, :], [mixT], [smix_s])

    if stage >= 3:
        P.flush()
        rb_sb = T("rb_sb", [32, 8], F32)
        oh_sb = Buf(cl.ap[0:32, 0:3, :].rearrange("p a b -> p (a b)"), "oh_sb")
        negf_sb = Buf(zs.ap[:, 3:6, :].rearrange("p a b -> p (a b)"), "negf_sb")
        rbB = Buf(zraw.ap[0:32, 0:8, 0:128], "rbB")
        Frow = Buf(zs.ap[:, 0:3, :].rearrange("p a b -> p (a b)"), "Frow")
        Bst = Buf(zs.ap[:, 6, :], "Bst")
        attn_setup()
        P.flush()
        memset("pool", zraw, zraw.ap, 0.0)
    if stage >= 1:
        for gi, (t0, nt) in enumerate(groups):
            phase1_proj(gi, t0, nt)
            if stage >= 2:
                phase1_rwkv(gi, t0, nt)
            if stage >= 3:
                phase1_rest(gi, t0, nt)


    P.flush()
    ph1.close()
    ph1b = ExitStack()
    P.es = ph1b
    if stage >= 5:
        pt_i = T("pt_i", [128, 64], I32)
        iota_i = T("iota_i", [128, 1], I32)
        idx_i = T("idx_i", [128, 64], I32)
        kpg = [T("kpg%d" % i, [128, 512], F32) for i in range(3)]
        SC = T("SC", [128, 64, 8], F32)
        PP = T("PP", [128, 64, 8], F32)
        biasfull = T("biasfull", [128, 64, 8], F32)
        selB = T("selB", [128, 8, 32], F32)
        qb = T("qb", [128, 512], F32)
        tmpk = T("tmpk", [128, 512], F32)
        Er = T("Er", [128, 128], F32)
        Zsel = T("Zsel", [128, 63], F32)
        rb2 = T("rb2", [32, 8], F32)
        ohs_sb = T("ohs_sb", [32, 128], F32)
        b0b = T("b0b", [128, 8], F32)
        bm8 = T("bm8", [8, 512], F32)
        ones8 = T("ones8", [8, 128], F32)
        gate_s = T("gate_s", [32, 8], F32)
        gT = T("gT", [8, 32], F32)
        gT2 = T("gT2", [8, 32], F32)
        eq8 = T("eq8", [8, 32], F32)
        mx8 = T("mx8", [8, 4], F32)
        Rexp = T("Rexp", [8, 8, 32], F32)
        pown = T("pown", [128, 8], F32)
        ones_c = T("ones_c", [128, 1], F32)
        Osb = T("Osb", [8, 512], F32)
        rden = T("rden", [8, 2], F32)
        xtb = T("xtb", [128, 1024], F32)
        smix = T("smix", [128, 8, 2, 128], BF16)
        sqkv = T("sqkv", [128, 2, 3, 512], F32)
        x1tb = T("x1tb", [128, 1024], F32)
        k.kpi = 0

        P.op("pool", lambda e: e.iota(iota_i.ap, [[0, 1]], base=0, channel_multiplier=1), reads=[], writes=[iota_i])
        memset("pool", Zsel, Zsel.ap, 0.0)
        memset("pool", Zsel, Zsel.ap[:, 31:32], 1.0 / 256)
        memset("pool", ones8, ones8.ap, 1.0)
        memset("pool", ones_c, ones_c.ap, 1.0)
        ld(rb2, rb2.ap, rb_d)
        ld(ohs_sb, ohs_sb.ap, c_ohs)
        ld(b0b, b0b.ap, rb_d[0, :].partition_broadcast(128))
        ld(bm8, bm8.ap, c_bm8)
        cp("dve", biasfull.ap, b31.ap.unsqueeze(1).to_broadcast([128, 64, 8]), [b31], [biasfull])
        bk = bank()
        mm(bk, bk.ap[:, 0:8], ohs_sb.ap, rb2.ap, [ohs_sb, rb2])
        cp("dve", biasfull.ap[:, 63, :], bk.ap[:, 0:8], [bk], [biasfull])

        def sample_attn(si, b, r):
            ld(pt_i, pt_i.ap, pt_d[b].partition_broadcast(128))
            stt(idx_i.ap, pt_i.ap, 128.0, iota_i.ap.to_broadcast([128, 64]), ALU.mult, ALU.add, [pt_i, iota_i], [idx_i])
            cp("dve", Er.ap, ident_f.ap[:, r:r + 1].to_broadcast([128, 128]), [ident_f], [Er])
            bk = bank()
            mm(bk, bk.ap[:, 0:512], Er.ap, sqkv.ap[:, si, 0, :], [Er, sqkv])
            cp("act", qb.ap, bk.ap[:, 0:512], [bk], [qb])
            KM = bank(reserve=True)
            for pg in range(64):
                kb = kpg[k.kpi % 3]
                k.kpi += 1
                P.dma("pool", lambda e, kb=kb, pg=pg: e.indirect_dma_start(
                    out=kb.ap, out_offset=None, in_=ck_d,
                    in_offset=bass.IndirectOffsetOnAxis(ap=idx_i.ap[:, pg:pg + 1], axis=0)), reads=[idx_i], writes=[kb])
                n = pg // 2
                mm(KM, KM.ap[0:32, 0:512], Zsel.ap[:, 31 - n:63 - n], kb.ap, [Zsel, kb], start=(pg == 0), stop=(pg == 63))
                tt("pool", tmpk.ap, kb.ap, qb.ap, ALU.mult, [kb, qb], [tmpk])
                red(SC.ap[:, pg, :], tmpk.ap.rearrange("p (h d) -> p h d", h=8), ALU.add, [tmpk], [SC])
            tt("dve", tmpk.ap[0:32, :], KM.ap[0:32, 0:512], qb.ap[0:32, :], ALU.mult, [KM, qb], [tmpk])
            release(KM)
            red(gate_s.ap, tmpk.ap[0:32, :].rearrange("p (h d) -> p h d", h=8), ALU.add, [tmpk], [gate_s])
            bk = bank()
            tr(bk, bk.ap[0:8, 0:32], gate_s.ap, ident_f.ap[0:32, 0:32], [gate_s, ident_f])
            cp("dve", gT.ap, bk.ap[0:8, 0:32], [bk], [gT])
            red(mx8.ap[:, 0:1], gT.ap, ALU.max, [gT], [mx8])
            tt("dve", eq8.ap, gT.ap, mx8.ap[:, 0:1].to_broadcast([8, 32]), ALU.is_ge, [gT, mx8], [eq8])
            stt(gT2.ap, eq8.ap, -3e30, gT.ap, ALU.mult, ALU.add, [eq8, gT], [gT2])
            red(mx8.ap[:, 1:2], gT2.ap, ALU.max, [gT2], [mx8])
            tt("dve", eq8.ap, gT2.ap, mx8.ap[:, 1:2].to_broadcast([8, 32]), ALU.is_ge, [gT2, mx8], [eq8])
            stt(gT2.ap, eq8.ap, -3e30, gT2.ap, ALU.mult, ALU.add, [eq8, gT2], [gT2])
            red(mx8.ap[:, 2:3], gT2.ap, ALU.max, [gT2], [mx8])
            tt("dve", eq8.ap, gT.ap, mx8.ap[:, 2:3].to_broadcast([8, 32]), ALU.is_ge, [gT, mx8], [eq8])
            tt("dve", Rexp.ap, eq8.ap.unsqueeze(1).to_broadcast([8, 8, 32]),
               bm8.ap.rearrange("p (h d) -> p h d", h=8)[:, :, 0:32], ALU.mult, [eq8, bm8], [Rexp])
            bk = bank()
            mm(bk, bk.ap[:, 0:256], ones8.ap, Rexp.ap.rearrange("p h n -> p (h n)"), [ones8, Rexp])
            cp("dve", selB.ap, bk.ap[:, 0:256].rearrange("p (h n) -> p h n", h=8), [bk], [selB])
            stt(PP.ap, SC.ap, 0.125, biasfull.ap, ALU.mult, ALU.add, [SC, biasfull], [PP])
            act(PP.ap, PP.ap, AF.Exp, [PP], [PP])
            tt("dve", PP.ap.rearrange("p (n t) h -> p n t h", t=2), PP.ap.rearrange("p (n t) h -> p n t h", t=2),
               selB.ap.rearrange("p h n -> p n h").unsqueeze(2).to_broadcast([128, 32, 2, 8]), ALU.mult, [PP, selB], [PP])
            tt("dve", tmpk.ap, sqkv.ap[:, si, 0, :], sqkv.ap[:, si, 1, :], ALU.mult, [sqkv], [tmpk])
            red(pown.ap, tmpk.ap.rearrange("p (h d) -> p h d", h=8), ALU.add, [tmpk], [pown])
            stt(pown.ap, pown.ap, 0.125, b0b.ap, ALU.mult, ALU.add, [pown, b0b], [pown])
            act(pown.ap, pown.ap, AF.Exp, [pown], [pown])
            ts("dve", pown.ap, pown.ap, ident_f.ap[:, r:r + 1], None, ALU.mult, None, [pown, ident_f], [pown])
            OB_ = bank(reserve=True)
            DB_ = bank(reserve=True)
            for pg in range(64):
                vb = kpg[k.kpi % 3]
                k.kpi += 1
                P.dma("pool", lambda e, vb=vb, pg=pg: e.indirect_dma_start(
                    out=vb.ap, out_offset=None, in_=cv_d,
                    in_offset=bass.IndirectOffsetOnAxis(ap=idx_i.ap[:, pg:pg + 1], axis=0)), reads=[idx_i], writes=[vb])
                mm(OB_, OB_.ap[0:8, 0:512], PP.ap[:, pg, :], vb.ap, [PP, vb], start=(pg == 0), stop=False)
                mm(DB_, DB_.ap[0:8, 0:1], PP.ap[:, pg, :], ones_c.ap, [PP, ones_c], start=(pg == 0), stop=False)
            mm(OB_, OB_.ap[0:8, 0:512], pown.ap, sqkv.ap[:, si, 2, :], [pown, sqkv], start=False, stop=True)
            mm(DB_, DB_.ap[0:8, 0:1], pown.ap, ones_c.ap, [pown, ones_c], start=False, stop=True)
            tt("dve", Osb.ap, OB_.ap[0:8, 0:512], bm8.ap, ALU.mult, [OB_, bm8], [Osb])
            P.op("dve", lambda e: e.reciprocal(out=rden.ap[:, 0:1], in_=DB_.ap[0:8, 0:1]), reads=[DB_], writes=[rden])
            release(OB_)
            release(DB_)
            bk = bank()
            for hp in range(4):
                mm(bk, bk.ap[:, hp:hp + 1], Osb.ap[:, hp * 128:(hp + 1) * 128], rden.ap[:, 0:1], [Osb, rden])
            cp("dve", smix.ap[:, 0:4, si, r], bk.ap[:, 0:4], [bk], [smix])

        for si in range(2):
            tile = 16 + si
            if tile not in [t0 for (t0, nt) in groups]:
                continue
            memset("pool", smix, smix.ap[:, 0:4, si, :], 0.0)
            ld(smix, smix.ap[:, 4:8, si, :], smix_s.ap[si], reads=[smix_s])
            for j3 in range(3):
                ld(sqkv, sqkv.ap[:, si, j3, :], sq_s.ap[si, j3], reads=[sq_s])
            for b, r in SMAP[tile]:
                sample_attn(si, b, r)
            ld(xtb, xtb.ap, x_d[tile * 128:(tile + 1) * 128, :])
            for hf in range(2):
                bk = bank()
                for c in range(8):
                    mm(bk, bk.ap[:, 0:512], smix.ap[:, c, si, :], wout_b.ap[:, c, hf * 512:(hf + 1) * 512], [smix, wout_b],
                       start=(c == 0), stop=(c == 7))
                tt("dve", x1tb.ap[:, hf * 512:(hf + 1) * 512], bk.ap[:, 0:512], xtb.ap[:, hf * 512:(hf + 1) * 512], ALU.add, [bk, xtb], [x1tb])
            st(x1_s.ap[tile * 128:(tile + 1) * 128, :], x1tb.ap, [x1tb], [x1_s])
    P.flush()
    ph1b.close()
    phS.close()

    ph2 = ExitStack()
    P.es = ph2
    ntl = [t0 for (t0, nt) in groups]
    NTT = 18
    h2T = T("h2T", [128, 8, NTT * 128], BF16)
    acc = T("acc", [128, NTT, 1024], F32)
    comb = T("comb", [128, NTT, 16], F32)
    fgb = T("fgb", [128, 1024], F32)
    ld(fgb, fgb.ap, fg_d.partition_broadcast(128))
    wpleg_b = T("wpleg_b", [128, 8, 1024], BF16)
    wple_b = T("wple_b", [128, 2, 1024], BF16)
    wr_b = T("wr_b", [128, 8, 20], BF16)
    wr_f = T("wr_f", [128, 8, 20], F32)
    brb = T("brb", [128, 20], F32)
    stg4 = T("stg4", [128, 2, 1024], F32)
    stg2 = Buf(stg4.ap.rearrange("p a b -> p (a b)").rearrange("p (c f) -> p c f", c=8), "stg2")
    weg_b = [T("weg_b%d" % i, [128, 8, 256], BF16) for i in range(2)]
    weu_b = [T("weu_b%d" % i, [128, 8, 256], BF16) for i in range(2)]
    wed_b = [T("wed_b%d" % i, [128, 2, 1024], BF16) for i in range(2)]
    actT = [T("actT%d" % i, [128, 2, 512], BF16) for i in range(2)]
    sil = T("sil", [128, 2, 512], F32)
    xs_b2 = T("xs_b2", [128, 1024], BF16)
    sqj2 = T("sqj2", [128, 1024], BF16)
    nst2 = T("nst2", [128, 8], F32)
    lg = T("lg", [128, 20], F32)
    rt = T("rt", [128, 16, 4], F32)
    rtm = T("rtm", [128, 16], F32)
    x2T = T("x2T", [128, 8, 128], BF16)
    pt_f = T("pt_f", [128, 256], F32)
    pt_b = T("pt_b", [128, 256], BF16)
    pT = T("pT", [128, 2, 128], BF16)
    sgt = T("sgt", [128, 1024], F32)
    yt = T("yt", [128, 1024], F32)

    for c in range(8):
        ld(stg4, stg4.ap[:, 0, :], wpleg_d[c * 128:(c + 1) * 128, :])
        cp("pool", wpleg_b.ap[:, c, :], stg4.ap[:, 0, :], [stg4], [wpleg_b])
    for c in range(2):
        ld(stg4, stg4.ap[:, 1, :], wple_d[c * 128:(c + 1) * 128, :])
        cp("pool", wple_b.ap[:, c, :], stg4.ap[:, 1, :], [stg4], [wple_b])
    ld(wr_f, wr_f.ap[:, :, 0:4], wrg_d.rearrange("(c p) g -> p c g", p=128), nc_ok=True)
    ld(wr_f, wr_f.ap[:, :, 4:20], wre_d.rearrange("(c p) g -> p c g", p=128), nc_ok=True)
    cp("dve", wr_b.ap, wr_f.ap, [wr_f], [wr_b])
    ld(brb, brb.ap[:, 0:4], brg_d.partition_broadcast(128))
    ld(brb, brb.ap[:, 4:20], bre_d.partition_broadcast(128))

    def rms2(src_ap, src_buf, gcol, dstT, dst_cols):
        act(sqj2.ap, src_ap, AF.Square, [src_buf], [sqj2, nst2], accum=nst2.ap[:, 0:1])
        ts("dve", nst2.ap[:, 1:2], nst2.ap[:, 0:1], 1.0 / 1024, 1e-6, ALU.mult, ALU.add, [nst2], [nst2])
        act(nst2.ap[:, 2:3], nst2.ap[:, 1:2], AF.Sqrt, [nst2], [nst2])
        P.op("dve", lambda e: e.reciprocal(out=nst2.ap[:, 3:4], in_=nst2.ap[:, 2:3]), reads=[nst2], writes=[nst2])

    def to_T(src_ap, src_buf, dstT, dst_cols, gcol=None, scale_col=None):
        if scale_col is not None:
            ts("dve", xs_b2.ap, src_ap, scale_col, None, ALU.mult, None, [src_buf, nst2], [xs_b2])
        else:
            cp("dve", xs_b2.ap, src_ap, [src_buf], [xs_b2])
        bk = bank()
        bv = bfv(bk)
        for c in range(8):
            tr(bk, bv[:, c * 128:(c + 1) * 128], xs_b2.ap[:, c * 128:(c + 1) * 128], ident_b.ap, [xs_b2, ident_b])
        if gcol is not None:
            tt("dve", dstT.ap[:, :, dst_cols], bv[:, 0:1024].rearrange("p (c t) -> p c t", c=8),
               gcol.ap.unsqueeze(2).to_broadcast([128, 8, 128]), ALU.mult, [bk, gcol], [dstT])
        else:
            cp("dve", dstT.ap[:, :, dst_cols], bv[:, 0:1024].rearrange("p (c t) -> p c t", c=8), [bk], [dstT])

    def route(tile, h2f):
        tc_ = slice(tile * 128, (tile + 1) * 128)
        bk = bank()
        for c in range(8):
            mm(bk, bk.ap[:, 0:20], h2f[:, c, :], wr_f.ap[:, c, :], [yt, wr_f], start=(c == 0), stop=(c == 7))
        tt("dve", lg.ap, bk.ap[:, 0:20], brb.ap, ALU.add, [bk, brb], [lg])
        G = rt.ap[:, 0, :]
        red(rtm.ap[:, 0:1], lg.ap[:, 0:4], ALU.max, [lg], [rtm])
        ts("dve", rtm.ap[:, 1:2], rtm.ap[:, 0:1], -1.0, None, ALU.mult, None, [rtm], [rtm])
        act(rt.ap[:, 1, :], lg.ap[:, 0:4], AF.Exp, [lg, rtm], [rt, rtm], bias=rtm.ap[:, 1:2], accum=rtm.ap[:, 2:3])
        P.op("dve", lambda e: e.reciprocal(out=rtm.ap[:, 3:4], in_=rtm.ap[:, 2:3]), reads=[rtm], writes=[rtm])
        tt("dve", G, lg.ap[:, 0:4], rtm.ap[:, 0:1].to_broadcast([128, 4]), ALU.is_ge, [lg, rtm], [rt])
        le = lg.ap[:, 4:20].rearrange("p (g e) -> p g e", g=4)
        tt("dve", rt.ap[:, 2:6, :], le, bc3(G, 4), ALU.mult, [lg, rt], [rt])
        red(rt.ap[:, 6, :], rt.ap[:, 2:6, :].rearrange("p g e -> p e g"), ALU.add, [rt], [rt])
        el = rt.ap[:, 6, :]
        red(rtm.ap[:, 4:5], el, ALU.max, [rt], [rtm])
        tt("dve", rt.ap[:, 7, :], el, rtm.ap[:, 4:5].to_broadcast([128, 4]), ALU.is_ge, [rt, rtm], [rt])
        stt(rt.ap[:, 8, :], rt.ap[:, 7, :], -3e30, el, ALU.mult, ALU.add, [rt], [rt])
        red(rtm.ap[:, 5:6], rt.ap[:, 8, :], ALU.max, [rt], [rtm])
        tt("dve", rt.ap[:, 9, :], rt.ap[:, 8, :], rtm.ap[:, 5:6].to_broadcast([128, 4]), ALU.is_ge, [rt, rtm], [rt])
        tt("dve", rtm.ap[:, 6:7], rtm.ap[:, 5:6], rtm.ap[:, 4:5], ALU.subtract, [rtm], [rtm])
        act(rtm.ap[:, 7:8], rtm.ap[:, 6:7], AF.Exp, [rtm], [rtm])
        ts("dve", rtm.ap[:, 8:9], rtm.ap[:, 7:8], 1.0, None, ALU.add, None, [rtm], [rtm])
        P.op("dve", lambda e: e.reciprocal(out=rtm.ap[:, 9:10], in_=rtm.ap[:, 8:9]), reads=[rtm], writes=[rtm])
        tt("dve", rtm.ap[:, 10:11], rtm.ap[:, 7:8], rtm.ap[:, 9:10], ALU.mult, [rtm], [rtm])
        tt("dve", rtm.ap[:, 9:10], rtm.ap[:, 9:10], rtm.ap[:, 3:4], ALU.mult, [rtm], [rtm])
        tt("dve", rtm.ap[:, 10:11], rtm.ap[:, 10:11], rtm.ap[:, 3:4], ALU.mult, [rtm], [rtm])
        ts("dve", rt.ap[:, 10, :], rt.ap[:, 7, :], rtm.ap[:, 9:10], None, ALU.mult, None, [rt, rtm], [rt])
        stt(rt.ap[:, 11, :], rt.ap[:, 9, :], rtm.ap[:, 10:11], rt.ap[:, 10, :], ALU.mult, ALU.add, [rt, rtm], [rt])
        tt("dve", comb.ap[:, tile, :].rearrange("p (g e) -> p g e", g=4), bc3(G, 4),
           rt.ap[:, 11, :].unsqueeze(1).to_broadcast([128, 4, 4]), ALU.mult, [rt], [comb])

    if stage >= 4:
        for tile in ntl:
            tc_ = slice(tile * 128, (tile + 1) * 128)
            ld(acc, acc.ap[:, tile, :], x1_s.ap[tile * 128:(tile + 1) * 128, :], reads=[x1_s])
            rms2(acc.ap[:, tile, :], acc, None, None, None)
            ts("dve", sgt.ap, acc.ap[:, tile, :], nst2.ap[:, 3:4], None, ALU.mult, None, [acc, nst2], [sgt])
            h2f = yt.ap.rearrange("p (c t) -> p c t", c=8)
            for half in range(2):
                bk = bank()
                for c4 in range(4):
                    c = half * 4 + c4
                    tr(bk, bk.ap[:, c4 * 128:(c4 + 1) * 128], sgt.ap[:, c * 128:(c + 1) * 128], ident_f.ap, [sgt, ident_f])
                tt("dve", h2f[:, half * 4:half * 4 + 4, :], bk.ap[:, 0:512].rearrange("p (c t) -> p c t", c=4),
                   g2c.ap[:, half * 4:half * 4 + 4].unsqueeze(2).to_broadcast([128, 4, 128]), ALU.mult, [bk, g2c], [yt])
            cp("pool", h2T.ap[:, :, tc_], h2f, [yt], [h2T])
            route(tile, h2f)
        tgs = []
        cur = []
        for tile in ntl:
            if cur and (tile != cur[-1] + 1 or len(cur) == 4):
                tgs.append(cur)
                cur = []
            cur.append(tile)
        if cur:
            tgs.append(cur)
        for e_ in range(N_EXP):
            wg = weg_b[e_ % 2]
            wu = weu_b[e_ % 2]
            wd = wed_b[e_ % 2]
            ld(stg4, stg2.ap, weg_d[e_].rearrange("(c p) f -> p c f", p=128))
            cp("pool", wg.ap, stg2.ap, [stg4], [wg])
            ld(stg4, stg2.ap, weu_d[e_].rearrange("(c p) f -> p c f", p=128))
            cp("pool", wu.ap, stg2.ap, [stg4], [wu])
            ld(stg4, stg4.ap, wed_d[e_].rearrange("(c p) d -> p c d", p=128))
            cp("dve", wd.ap, stg4.ap, [stg4], [wd])
            for tg in tgs:
                n_ = len(tg) * 128
                cs_ = slice(tg[0] * 128, tg[0] * 128 + n_)
                at = actT[k.pti % 2]
                k.pti += 1
                for fc in range(2):
                    bg_ = bank()
                    bu_ = bank()
                    for c in range(8):
                        mm(bg_, bg_.ap[:, 0:n_], wg.ap[:, c, fc * 128:(fc + 1) * 128], h2T.ap[:, c, cs_], [wg, h2T], start=(c == 0), stop=(c == 7))
                    for c in range(8):
                        mm(bu_, bu_.ap[:, 0:n_], wu.ap[:, c, fc * 128:(fc + 1) * 128], h2T.ap[:, c, cs_], [wu, h2T], start=(c == 0), stop=(c == 7))
                    act(sil.ap[:, fc, 0:n_], bg_.ap[:, 0:n_], AF.Silu, [bg_], [sil])
                    tt("dve", at.ap[:, fc, 0:n_], sil.ap[:, fc, 0:n_], bu_.ap[:, 0:n_], ALU.mult, [sil, bu_], [at])
                for i, tile in enumerate(tg):
                    for dh in range(2):
                        by = bank()
                        for fc in range(2):
                            mm(by, by.ap[:, 0:512], at.ap[:, fc, i * 128:(i + 1) * 128], wd.ap[:, fc, dh * 512:(dh + 1) * 512], [at, wd],
                               start=(fc == 0), stop=(fc == 1))
                        stt(acc.ap[:, tile, dh * 512:(dh + 1) * 512], by.ap[:, 0:512], comb.ap[:, tile, e_:e_ + 1],
                            acc.ap[:, tile, dh * 512:(dh + 1) * 512], ALU.mult, ALU.add, [by, comb, acc], [acc])
        for tile in ntl:
            x2 = acc.ap[:, tile, :]
            to_T(x2, acc, x2T, slice(0, 128))
            ld(pt_f, pt_f.ap, p_d[tile * 128:(tile + 1) * 128, :])
            cp("pool", pt_b.ap, pt_f.ap, [pt_f], [pt_b])
            bk = bank()
            bv = bfv(bk)
            for c in range(2):
                tr(bk, bv[:, c * 128:(c + 1) * 128], pt_b.ap[:, c * 128:(c + 1) * 128], ident_b.ap, [pt_b, ident_b])
            cp("act", pT.ap, bv[:, 0:256].rearrange("p (c t) -> p c t", c=2), [bk], [pT])
            for dh in range(2):
                dsl = slice(dh * 512, (dh + 1) * 512)
                bgt = bank()
                for c in range(8):
                    mm(bgt, bgt.ap[:, 0:512], x2T.ap[:, c, :], wpleg_b.ap[:, c, dsl], [x2T, wpleg_b], start=(c == 0), stop=(c == 7))
                act(sgt.ap[:, dsl], bgt.ap[:, 0:512], AF.Sigmoid, [bgt], [sgt])
                bpe = bank()
                for c in range(2):
                    mm(bpe, bpe.ap[:, 0:512], pT.ap[:, c, :], wple_b.ap[:, c, dsl], [pT, wple_b], start=(c == 0), stop=(c == 1))
                tt("dve", sgt.ap[:, dsl], sgt.ap[:, dsl], bpe.ap[:, 0:512], ALU.mult, [sgt, bpe], [sgt])
            tt("pool", yt.ap, sgt.ap, x2, ALU.add, [sgt, acc], [yt])
            rms2(yt.ap, yt, None, None, None)
            stt(yt.ap, yt.ap, nst2.ap[:, 3:4], fgb.ap, ALU.mult, ALU.mult, [yt, nst2, fgb], [yt])
            st(y_o[tile * 128:(tile + 1) * 128, :], yt.ap, [yt])

    P.emit()
    ph2.close()
    return k


def core_inputs(inp, c, consts=None):
    f = lambda a: np.ascontiguousarray(np.asarray(a))
    consts = consts if consts is not None else host_consts()
    x = np.zeros((NT_ALL * 128, D_MODEL), np.float32)
    p = np.zeros((NT_ALL * 128, PLE_DIM), np.float32)
    x[:SEQ] = np.asarray(inp["x_prompt"])[c]
    p[:SEQ] = np.asarray(inp["p_prompt"])[0, c]
    for b in range(4):
        x[SPOS[b]] = np.asarray(inp["x_sample"])[4 * c + b, 0]
        p[SPOS[b]] = np.asarray(inp["p_sample"])[0, 4 * c + b, 0]
    m = {
        "x": x, "p": p,
        "cache_k": np.asarray(inp["cache_k"]).reshape(2560 * 128, 512),
        "cache_v": np.asarray(inp["cache_v"]).reshape(2560 * 128, 512),
        "page_table": f(np.asarray(inp["page_table"])[4 * c:4 * c + 4]).astype(np.int32),
        "state_wkv": f(np.asarray(inp["state_wkv"])[0, 4 * c:4 * c + 4]),
        "state_shift": f(np.asarray(inp["state_shift"])[0, 4 * c:4 * c + 4]),
        "rel_bias": f(inp["rel_bias"]),
        "final_g": f(inp["final_g"]),
        "r_k": f(np.asarray(inp["r_k"])[0]).reshape(W_R),
    }
    for nm in ("norm1_g", "w_in", "shift_mu", "w0", "w2", "a0", "a2", "g2", "k_k", "k_a", "lnx_g", "lnx_b",
               "w_out", "norm2_g", "w_rg", "b_rg", "w_re", "b_re", "w_eg", "w_eu", "w_ed", "w_ple", "w_pleg"):
        m[nm] = f(np.asarray(inp[nm])[0])
    m.update(consts)
    return m


def assemble(res):
    y_p = np.zeros((8, SEQ, D_MODEL), np.float32)
    y_s = np.zeros((32, 1, D_MODEL), np.float32)
    k_p = np.zeros((1, 8, SEQ, 8, 64), np.float32)
    v_p = np.zeros((1, 8, SEQ, 8, 64), np.float32)
    k_s = np.zeros((1, 32, 1, 8, 64), np.float32)
    v_s = np.zeros((1, 32, 1, 8, 64), np.float32)
    w_p = np.zeros((1, 8, 8, 64, 64), np.float32)
    w_s = np.zeros((1, 32, 8, 64, 64), np.float32)
    s_p = np.zeros((1, 8, RW_COLS), np.float32)
    s_s = np.zeros((1, 32, RW_COLS), np.float32)
    for c, r in enumerate(res):
        y_p[c] = r["y"][:SEQ]
        k_p[0, c] = r["k_new"][:SEQ].reshape(SEQ, 8, 64)
        v_p[0, c] = r["v_new"][:SEQ].reshape(SEQ, 8, 64)
        w_p[0, c] = r["wkv_p"]
        s_p[0, c] = r["shift_p"]
        for b in range(4):
            y_s[4 * c + b, 0] = r["y"][SPOS[b]]
            k_s[0, 4 * c + b, 0] = r["k_new"][SPOS[b]].reshape(8, 64)
            v_s[0, 4 * c + b, 0] = r["v_new"][SPOS[b]].reshape(8, 64)
            w_s[0, 4 * c + b] = r["wkv_s"][b]
            s_s[0, 4 * c + b] = r["shift_s"][b]
    return (y_p, y_s, k_p, v_p, k_s, v_s, w_p, w_s, s_p, s_s)


def kernel(**inputs):
    nc = bass.Bass("TRN2", target_bir_lowering=False)
    with ExitStack() as es:
        k = build(nc, es)
    consts = host_consts()
    in_maps = [core_inputs(inputs, c, consts) for c in range(8)]
    res = run_bass_kernel_spmd(nc, in_maps, core_ids=list(range(8)))
    return assemble(res.results)
```

```python
import numpy as np
import concourse.bass as bass
import concourse.mybir as mybir
from concourse.bass_utils import run_bass_kernel_spmd
from contextlib import ExitStack

F32 = mybir.dt.float32
BF16 = mybir.dt.bfloat16
I32 = mybir.dt.int32
AF = mybir.ActivationFunctionType
ALU = mybir.AluOpType
AX = mybir.AxisListType

NDS = 48
NDP = 16


class Buf:
    __slots__ = ("ap", "w", "r", "name")

    def __init__(self, ap, name=""):
        self.ap = ap
        self.w = None
        self.r = []
        self.name = name

    def __getitem__(self, k):
        return self.ap[k]


class Prog:
    ENGS = ("pe", "dve", "act", "pool", "sp")

    def __init__(self, nc, es):
        self.nc = nc
        self.es = es
        self.q = {e: [] for e in self.ENGS}
        self.cnt = {e: 0 for e in self.ENGS}
        self.sem = {e: es.enter_context(nc.semaphore("s_" + e)) for e in self.ENGS}
        self.dsem = [es.enter_context(nc.semaphore("d%d" % i)) for i in range(NDS + NDP)]
        self.dcnt = [0] * (NDS + NDP)
        self.di = 0
        self.dpi = 0
        self.seen = {e: {} for e in self.ENGS}
        self.nalloc = 0

    def sb(self, name, shape, dt):
        t = self.es.enter_context(self.nc.sbuf_tensor(name, list(shape), dt))
        return Buf(t.ap(), name)

    def ps(self, name, shape, dt=F32):
        t = self.es.enter_context(self.nc.psum_tensor(name, list(shape), dt))
        return Buf(t.ap(), name)

    def view(self, ap, name=""):
        return Buf(ap, name)

    def _deps(self, tok, eng, reads, writes):
        waits = []
        for b in reads:
            if b.w is not None:
                waits.append((b.w, True))
        for b in writes:
            if b.w is not None:
                waits.append((b.w, False))
            for t in b.r:
                waits.append((t, False))
        for b in reads:
            b.r.append(tok)
        for b in writes:
            b.w = tok
            b.r = []
        out = []
        for t, raw in waits:
            if t[0] == "c" and t[1] == eng and tok[0] == "c" and eng == "pe":
                continue
            out.append(t)
        return out

    def op(self, eng, fn, reads=(), writes=()):
        idx = self.cnt[eng] + 1
        self.cnt[eng] = idx
        tok = ("c", eng, idx)
        waits = self._deps(tok, eng, reads, writes)
        self.q[eng].append((waits, fn, None))
        return tok

    def dma(self, eng, fn, reads=(), writes=()):
        if eng == "pool":
            slot = NDS + self.dpi % NDP
            self.dpi += 1
        else:
            slot = self.di % NDS
            self.di += 1
        prev = self.dcnt[slot]
        self.dcnt[slot] = prev + 1
        tok = ("d", slot, prev + 1)
        waits = self._deps(tok, eng, reads, writes)
        if prev > 0:
            waits.append(("d", slot, prev))
        self.q[eng].append((waits, fn, slot))
        return tok

    def _semval(self, t):
        if t[0] == "c":
            return self.sem[t[1]], t[2], ("c", t[1])
        return self.dsem[t[1]], 16 * t[2], ("d", t[1])

    def flush(self, final=False):
        nc = self.nc
        if final:
            fin = [("d", s, c) for s, c in enumerate(self.dcnt) if c > 0]
            self.q["sp"].append((fin, None, None))
        else:
            toks = [("c", e, self.cnt[e]) for e in self.ENGS if self.cnt[e] > 0]
            toks += [("d", s, c) for s, c in enumerate(self.dcnt) if c > 0]
            for e in self.ENGS:
                self.q[e].append((list(toks), None, None))
        qs = self.q
        self.q = {e: [] for e in self.ENGS}
        with nc.Block() as block:
            def run(engname):
                def body(e):
                    seen = self.seen[engname]
                    for waits, fn, slot in qs[engname]:
                        for t in waits:
                            sem, val, key = self._semval(t)
                            if seen.get(key, 0) >= val:
                                continue
                            seen[key] = val
                            e.wait_ge(sem, val)
                        if fn is None:
                            continue
                        ins = fn(e)
                        if slot is None:
                            ins.then_inc(self.sem[engname], 1)
                        else:
                            ins.then_inc(self.dsem[slot], 16)
                return body
            block.tensor(run("pe"))
            block.vector(run("dve"))
            block.scalar(run("act"))
            block.gpsimd(run("pool"))
            block.sync(run("sp"))

    def emit(self):
        self.flush(final=True)


D_MODEL = 1024
SEQ = 2048
NTILE = 16
NT_ALL = 18
W_A = 512
W_R = 512
RW_COLS = 1824
IN_COLS = 3360
N_EXP = 16
D_EXP = 256
PLE_DIM = 256
N_PAGES = 64
SMAP = {16: ((0, 0), (1, 32), (2, 64)), 17: ((3, 0),)}
SPOS = {0: 2048 + 0, 1: 2048 + 32, 2: 2048 + 64, 3: 2048 + 128}
GROUPS = [(i, 1) for i in range(18)]
NEGB = -240000.0


def _rel_bucket_np(dist):
    n = np.maximum(dist, 0)
    nf = np.maximum(n, 1).astype(np.float32)
    large = 16 + (np.log(nf / 16) / np.log(np.float32(128 / 16)) * 16).astype(np.int32)
    large = np.minimum(large, 31)
    return np.where(n < 16, n, large)


def host_consts():
    c = {}
    c["c_ident"] = np.eye(128, dtype=np.float32)
    r = np.arange(128)[:, None]
    q = np.arange(128)[None, :]
    m = np.zeros((128, 3, 128), np.float32)
    m[:, 0, :] = (r < q)
    m[:, 1, :] = (r <= q)
    m[:, 2, :] = (r > q)
    c["c_masks"] = m
    c["c_bones"] = ((r // 64) == (q // 64)).astype(np.float32)
    ind = np.zeros((128, 2), np.float32)
    ind[:64, 0] = 1
    ind[64:, 1] = 1
    c["c_ind"] = ind
    cc = np.arange(384)
    oh = np.zeros((32, 384), np.float32)
    bk = _rel_bucket_np(cc - 127)
    for i in range(384):
        if cc[i] >= 127:
            oh[bk[i], i] = 1.0
    c["c_oh"] = oh
    negf = np.zeros((128, 384), np.float32)
    negf[:, :127] = NEGB
    c["c_negf"] = negf
    ohs = np.zeros((32, 128), np.float32)
    bks = _rel_bucket_np(128 - np.arange(128))
    ohs[bks, np.arange(128)] = 1.0
    c["c_ohs"] = ohs
    bm8 = np.zeros((8, 8, 64), np.float32)
    for h in range(8):
        bm8[h, h, :] = 1.0
    c["c_bm8"] = bm8.reshape(8, 512)
    gm = np.zeros((4, 8, 8), np.float32)
    for a in range(4):
        gm[a, :, a + 4:] = -1e30
    c["c_nio"] = np.tile(gm.reshape(1, 256), (128, 1))
    return c


class K:
    pass


def build(nc, es, stage=99, dbg=False, groups=None):
    groups = GROUPS if groups is None else groups
    P = Prog(nc, es)
    k = K()
    k.P = P

    def DI(name, shape, dt=F32):
        return nc.dram_tensor(name, list(shape), dt, kind="ExternalInput").ap()

    def DO(name, shape, dt=F32):
        return nc.dram_tensor(name, list(shape), dt, kind="ExternalOutput").ap()

    def DS(name, shape, dt=F32):
        return Buf(nc.dram_tensor(name, list(shape), dt, kind="Internal").ap(), name)

    x_d = DI("x", [NT_ALL * 128, D_MODEL])
    p_d = DI("p", [NT_ALL * 128, PLE_DIM])
    ck_d = DI("cache_k", [2560 * 128, 512])
    cv_d = DI("cache_v", [2560 * 128, 512])
    pt_d = DI("page_table", [4, N_PAGES], I32)
    swkv_d = DI("state_wkv", [4, 8, 64, 64])
    ssh_d = DI("state_shift", [4, RW_COLS])
    n1_d = DI("norm1_g", [D_MODEL])
    win_d = DI("w_in", [D_MODEL, IN_COLS])
    rb_d = DI("rel_bias", [32, 8])
    mu_d = DI("shift_mu", [RW_COLS])
    w0_d = DI("w0", [W_R])
    w2_d = DI("w2", [64, W_R])
    a0_d = DI("a0", [W_R])
    a2_d = DI("a2", [64, W_R])
    g2_d = DI("g2", [160, W_R])
    kk_d = DI("k_k", [W_R])
    ka_d = DI("k_a", [W_R])
    rk_d = DI("r_k", [W_R])
    lg_d = DI("lnx_g", [W_R])
    lb_d = DI("lnx_b", [W_R])
    wout_d = DI("w_out", [1024, D_MODEL])
    n2_d = DI("norm2_g", [D_MODEL])
    wrg_d = DI("w_rg", [D_MODEL, 4])
    brg_d = DI("b_rg", [4])
    wre_d = DI("w_re", [D_MODEL, 16])
    bre_d = DI("b_re", [16])
    weg_d = DI("w_eg", [N_EXP, D_MODEL, D_EXP])
    weu_d = DI("w_eu", [N_EXP, D_MODEL, D_EXP])
    wed_d = DI("w_ed", [N_EXP, D_EXP, D_MODEL])
    wple_d = DI("w_ple", [PLE_DIM, D_MODEL])
    wpleg_d = DI("w_pleg", [D_MODEL, D_MODEL])
    fg_d = DI("final_g", [D_MODEL])
    c_ident = DI("c_ident", [128, 128])
    c_masks = DI("c_masks", [128, 3, 128])
    c_bones = DI("c_bones", [128, 128])
    c_ind = DI("c_ind", [128, 2])
    c_oh = DI("c_oh", [32, 384])
    c_negf = DI("c_negf", [128, 384])
    c_ohs = DI("c_ohs", [32, 128])
    c_bm8 = DI("c_bm8", [8, 512])
    c_nio = DI("c_nio", [128, 256])

    y_o = DO("y", [NT_ALL * 128, D_MODEL])
    kp_o = DO("k_new", [NT_ALL * 128, 512])
    vp_o = DO("v_new", [NT_ALL * 128, 512])
    wkvp_o = DO("wkv_p", [8, 64, 64])
    wkvs_o = DO("wkv_s", [4, 8, 64, 64])
    shp_o = DO("shift_p", [RW_COLS])
    shs_o = DO("shift_s", [4, RW_COLS])

    x1_s = DS("x1_scr", [NT_ALL * 128, D_MODEL])
    bias_s = DS("bias_scr", [8, 128 * 384])
    sq_s = DS("sq_scr", [2, 3, 128, 512])
    smix_s = DS("smix_scr", [2, 128, 4, 128], BF16)

    banks = [P.ps("pb%d" % i, [128, 512], F32) for i in range(8)]
    k.bi = 0

    k.reserved = set()

    def bank(reserve=False):
        while True:
            i = k.bi % 8
            k.bi += 1
            if i not in k.reserved:
                break
        if reserve:
            k.reserved.add(i)
        return banks[i]

    def release(b):
        k.reserved.discard(banks.index(b))

    def mm(bk, out, lhsT, rhs, reads, start=True, stop=True):
        P.op("pe", lambda e: e.matmul(out, lhsT, rhs, start=start, stop=stop), reads=reads, writes=[bk])

    def tr(bk, out, in_, ident, reads):
        P.op("pe", lambda e: e.transpose(out, in_, ident), reads=reads, writes=[bk])

    def act(out, in_, func, reads, writes, bias=None, scale=None, accum=None, eng="act"):
        kw = {}
        if bias is not None:
            kw["bias"] = bias
        if scale is not None:
            kw["scale"] = scale
        if accum is not None:
            kw["accum_out"] = accum
        P.op("act", lambda e: e.activation(out=out, in_=in_, func=func, **kw), reads=reads, writes=writes)

    def tt(eng, out, in0, in1, op, reads, writes):
        P.op(eng, lambda e: e.tensor_tensor(out=out, in0=in0, in1=in1, op=op), reads=reads, writes=writes)

    def ts(eng, out, in0, s1, s2, op0, op1, reads, writes):
        if op1 is None:
            P.op(eng, lambda e: e.tensor_scalar(out=out, in0=in0, scalar1=s1, scalar2=None, op0=op0),
                 reads=reads, writes=writes)
        else:
            P.op(eng, lambda e: e.tensor_scalar(out=out, in0=in0, scalar1=s1, scalar2=s2, op0=op0, op1=op1),
                 reads=reads, writes=writes)

    def stt(out, in0, scalar, in1, op0, op1, reads, writes):
        P.op("dve", lambda e: e.scalar_tensor_tensor(out=out, in0=in0, scalar=scalar, in1=in1, op0=op0, op1=op1),
             reads=reads, writes=writes)

    def cp(eng, out, in_, reads, writes):
        if eng == "act":
            P.op("act", lambda e: e.copy(out=out, in_=in_), reads=reads, writes=writes)
        else:
            P.op(eng, lambda e: e.tensor_copy(out=out, in_=in_), reads=reads, writes=writes)

    def red(out, in_, op, reads, writes, axis=AX.X):
        P.op("dve", lambda e: e.tensor_reduce(out=out, in_=in_, axis=axis, op=op), reads=reads, writes=writes)

    def ld(out_buf, out, in_, eng="sp", reads=(), nc_ok=False):
        if nc_ok:
            P.dma(eng, lambda e: e.dma_start(out=out, in_=in_, allow_slow_non_contiguous=True), reads=list(reads), writes=[out_buf])
        else:
            P.dma(eng, lambda e: e.dma_start(out=out, in_=in_), reads=list(reads), writes=[out_buf])

    def st(out, in_, reads, writes=(), eng="sp", nc_ok=False):
        if nc_ok:
            P.dma(eng, lambda e: e.dma_start(out=out, in_=in_, allow_slow_non_contiguous=True), reads=list(reads), writes=list(writes))
        else:
            P.dma(eng, lambda e: e.dma_start(out=out, in_=in_), reads=list(reads), writes=list(writes))

    def memset(eng, buf, ap, val):
        P.op(eng, lambda e: e.memset(ap, val), reads=[], writes=[buf])

    def bfv(b):
        return b.ap.bitcast(BF16)

    ident_f = P.sb("ident_f", [128, 128], F32)
    ident_b = P.sb("ident_b", [128, 128], BF16)
    masks = P.sb("masks", [128, 3, 128], F32)
    bones_f = P.sb("bones_f", [128, 128], F32)
    bones_b = P.sb("bones_b", [128, 128], BF16)
    ind_f = P.sb("ind_f", [128, 2], F32)
    ind_b = P.sb("ind_b", [128, 2], BF16)
    ld(ident_f, ident_f.ap, c_ident)
    ld(masks, masks.ap, c_masks)
    ld(bones_f, bones_f.ap, c_bones)
    ld(ind_f, ind_f.ap, c_ind)
    nio = P.sb("nio", [128, 4, 64], F32)
    ld(nio, nio.ap.rearrange("p a n -> p (a n)"), c_nio)
    cp("dve", ident_b.ap, ident_f.ap, [ident_f], [ident_b])
    cp("dve", bones_b.ap, bones_f.ap, [bones_f], [bones_b])
    cp("dve", ind_b.ap, ind_f.ap, [ind_f], [ind_b])

    g1c = P.sb("g1c", [128, 8], F32)
    g2c = P.sb("g2c", [128, 8], F32)
    ld(g1c, g1c.ap, n1_d.rearrange("(c p) -> p c", p=128), nc_ok=True)
    ld(g2c, g2c.ap, n2_d.rearrange("(c p) -> p c", p=128), nc_ok=True)
    muc = P.sb("muc", [128, 15], F32)
    memset("pool", muc, muc.ap, 0.0)
    ld(muc, muc.ap[:, 0:14], mu_d[0:1792].rearrange("(c p) -> p c", p=128), nc_ok=True)
    ld(muc, muc.ap[0:32, 14:15], mu_d[1792:1824].rearrange("(c p) -> p c", p=32), nc_ok=True)
    prm = {}
    for nm, d in (("w0", w0_d), ("a0", a0_d), ("kk", kk_d), ("ka", ka_d), ("rk", rk_d)):
        t = P.sb("prm_" + nm, [128, 4], F32)
        ld(t, t.ap, d.rearrange("(c p) -> p c", p=128), nc_ok=True)
        prm[nm] = t
    omka = P.sb("omka", [128, 4], F32)
    ts("dve", omka.ap, prm["ka"].ap, -1.0, 1.0, ALU.mult, ALU.add, [prm["ka"]], [omka])
    lgb = P.sb("lgb", [128, 512], F32)
    lbb = P.sb("lbb", [128, 512], F32)
    ld(lgb, lgb.ap, lg_d.partition_broadcast(128))
    ld(lbb, lbb.ap, lb_d.partition_broadcast(128))
    b31 = P.sb("b31", [128, 8], F32)
    ld(b31, b31.ap, rb_d[31, :].partition_broadcast(128))

    phS = ExitStack()
    P.es = phS
    wout_b = P.sb("wout_b", [128, 8, 1024], BF16)
    ph1 = ExitStack()
    P.es = ph1
    win_b = P.sb("win_b", [128, 8, IN_COLS], BF16)
    wa2_b = P.sb("wa2_b", [128, 512], BF16)
    g2a_b = P.sb("g2a_b", [128, 512], BF16)
    g2b_b = P.sb("g2b_b", [32, 512], BF16)
    ph0 = ExitStack()
    P.es = ph0
    stg = [P.sb("stg%d" % i, [128, 1680], F32) for i in range(2)]
    k.si = 0

    def load_cast(dst_buf, dst_ap, src_ap, shape_cols, eng="pool"):
        s = stg[k.si % 2]
        k.si += 1
        sv = s.ap[0:dst_ap.shape[0], 0:shape_cols]
        ld(s, sv, src_ap)
        if len(dst_ap.shape) == 3:
            sv = sv.rearrange("p (a b) -> p a b", a=dst_ap.shape[1])
        cp(eng, dst_ap, sv, [s], [dst_buf])

    for c in range(8):
        for hf in range(2):
            load_cast(win_b, win_b.ap[:, c, hf * 1680:(hf + 1) * 1680], win_d[c * 128:(c + 1) * 128, hf * 1680:(hf + 1) * 1680],
                      1680, eng=("pool" if hf else "dve"))
    for c in range(8):
        load_cast(wout_b, wout_b.ap[:, c, :], wout_d[c * 128:(c + 1) * 128, :], 1024)
    load_cast(wa2_b, wa2_b.ap[0:64, :], w2_d, 512)
    s = stg[k.si % 2]; k.si += 1
    ld(s, s.ap[64:128, 0:512], a2_d)
    cp("pool", wa2_b.ap[64:128, :], s.ap[64:128, 0:512], [s], [wa2_b])
    load_cast(g2a_b, g2a_b.ap, g2_d[0:128, :], 512)
    load_cast(g2b_b, g2b_b.ap, g2_d[128:160, :], 512)


    P.flush()
    ph0.close()
    P.es = ph1
    qT_g = P.sb("qT_g", [128, 4, 128], BF16)
    kT_all = P.sb("kT_all", [128, 4, 2048], BF16)
    qT_s = P.sb("qT_s", [128, 4, 128], BF16)
    kT_s = P.sb("kT_s", [128, 4, 128], BF16)
    kmT = P.sb("kmT", [128, 4, 8], F32)
    kmT_b = P.sb("kmT_b", [128, 4, 8], BF16)
    memset("pool", kmT, kmT.ap, 0.0)
    memset("pool", kmT_b, kmT_b.ap, 0.0)
    v_aug = P.sb("v_aug", [128, NTILE, 8, 66], BF16)
    memset("pool", v_aug, v_aug.ap, 1.0)
    zraw = P.sb("zraw", [128, 15, 129], F32)
    memset("pool", zraw, zraw.ap, 0.0)
    carry = P.sb("carry", [128, 15], F32)
    hT = P.sb("hT", [128, 8, 128], BF16)
    xt = [P.sb("xt%d" % i, [128, 1024], F32) for i in range(1)]
    k.xi = 0
    xs_b = P.sb("xs_b", [128, 1024], BF16)
    sqj = P.sb("sqj", [128, 1024], BF16)
    nst = P.sb("nst", [128, 8], F32)
    kvst = [P.sb("kvst%d" % i, [128, 2, 512], F32) for i in range(1)]
    k.kvi = 0

    def rms_to_T(src_ap, src_buf, gcol, dstT, dst_cols, stat):
        act(sqj.ap, src_ap, AF.Square, [src_buf], [sqj, stat], accum=stat.ap[:, 0:1])
        ts("dve", stat.ap[:, 1:2], stat.ap[:, 0:1], 1.0 / 1024, 1e-6, ALU.mult, ALU.add, [stat], [stat])
        act(stat.ap[:, 2:3], stat.ap[:, 1:2], AF.Sqrt, [stat], [stat])
        P.op("dve", lambda e: e.reciprocal(out=stat.ap[:, 3:4], in_=stat.ap[:, 2:3]), reads=[stat], writes=[stat])
        ts("dve", xs_b.ap, src_ap, stat.ap[:, 3:4], None, ALU.mult, None, [src_buf, stat], [xs_b])
        bk = bank()
        bv = bfv(bk)
        for c in range(8):
            tr(bk, bv[:, c * 128:(c + 1) * 128], xs_b.ap[:, c * 128:(c + 1) * 128], ident_b.ap, [xs_b, ident_b])
        tt("dve", dstT.ap[:, :, dst_cols], bv[:, 0:1024].rearrange("p (c t) -> p c t", c=8),
           gcol.ap.unsqueeze(2).to_broadcast([128, 8, 128]), ALU.mult, [bk, gcol], [dstT])

    def phase1_proj(gi, t0, nt):
        ntok = nt * 128
        tok0 = t0 * 128
        sample = (t0 >= 16)
        for i in range(nt):
            tile = t0 + i
            xb = xt[0]
            k.xi += 1
            ld(xb, xb.ap, x_d[tile * 128:(tile + 1) * 128, :])
            rms_to_T(xb.ap, xb, g1c, hT, slice(i * 128, (i + 1) * 128), nst)
        import os
        SKIP = os.environ.get('DBG_SKIP', '')
        for hp in range(0 if 'qk' in SKIP else 4):
            for which in range(2):
                col0 = which * 512 + hp * 128
                bk = bank()
                for c in range(8):
                    mm(bk, bk.ap[:, 0:ntok], win_b.ap[:, c, col0:col0 + 128], hT.ap[:, c, 0:ntok], [win_b, hT],
                       start=(c == 0), stop=(c == 7))
                if which == 0:
                    dst = qT_s if sample else qT_g
                    dcols = slice(0, ntok)
                else:
                    dst = kT_s if sample else kT_all
                    dcols = slice(0, 128) if sample else slice(tok0, tok0 + ntok)
                if which == 1 and not sample:
                    blk = t0 // 2
                    act(dst.ap[:, hp, dcols], bk.ap[:, 0:ntok], AF.Identity, [bk], [dst, nst], accum=nst.ap[:, 4 + hp:5 + hp])
                    if t0 % 2 == 0:
                        cp("dve", kmT.ap[:, hp, blk:blk + 1], nst.ap[:, 4 + hp:5 + hp], [nst], [kmT])
                    else:
                        tt("dve", kmT.ap[:, hp, blk:blk + 1], kmT.ap[:, hp, blk:blk + 1], nst.ap[:, 4 + hp:5 + hp], ALU.add, [kmT, nst], [kmT])
                else:
                    cp("act", dst.ap[:, hp, dcols], bk.ap[:, 0:ntok], [bk], [dst])
        if not sample and t0 % 2 == 1 and 'red' not in SKIP:
            blk = t0 // 2
            ts("dve", kmT_b.ap[:, :, blk:blk + 1], kmT.ap[:, :, blk:blk + 1], 1.0 / 256, None, ALU.mult, None, [kmT], [kmT_b])
        if gi > 0 and not sample:
            cp("pool", zraw.ap[:, :, 0], carry.ap, [carry], [zraw])
        for ct in range(0 if 'zz' in SKIP else 15):
            m = 128 if ct < 14 else 32
            col0 = 1536 + ct * 128
            bk = bank()
            for c in range(8):
                mm(bk, bk.ap[0:m, 0:ntok], win_b.ap[:, c, col0:col0 + m], hT.ap[:, c, 0:ntok], [win_b, hT],
                   start=(c == 0), stop=(c == 7))
            cp("act" if ct % 2 else "dve", zraw.ap[0:m, ct, 1:1 + ntok], bk.ap[0:m, 0:ntok], [bk], [zraw])
        import os
        SKIP = os.environ.get('DBG_SKIP', '')
        if 'shift' in SKIP:
            pass
        elif sample:
            for b, r in SMAP[t0]:
                st(shs_o[b, 0:1792].rearrange("(c p) -> p c", p=128), zraw.ap[:, 0:14, 1 + r], [zraw], nc_ok=True)
                st(shs_o[b, 1792:1824].rearrange("(c p) -> p c", p=32), zraw.ap[0:32, 14:15, 1 + r], [zraw], nc_ok=True)
            for b, r in SMAP[t0]:
                ld(zraw, zraw.ap[:, 0:14, r], ssh_d[b, 0:1792].rearrange("(c p) -> p c", p=128), nc_ok=True)
                ld(zraw, zraw.ap[0:32, 14:15, r], ssh_d[b, 1792:1824].rearrange("(c p) -> p c", p=32), nc_ok=True)
        else:
            cp("pool", carry.ap, zraw.ap[:, :, ntok], [zraw], [carry])
            if t0 + nt == 16:
                st(shp_o[0:1792].rearrange("(c p) -> p c", p=128), carry.ap[:, 0:14], [carry], nc_ok=True)
                st(shp_o[1792:1824].rearrange("(c p) -> p c", p=32), carry.ap[0:32, 14:15], [carry], nc_ok=True)
        for i in range(0 if 'kv' in SKIP else nt):
            tile = t0 + i
            bkk = bank()
            bkv = bank()
            for c in range(8):
                mm(bkk, bkk.ap[:, 0:512], hT.ap[:, c, i * 128:(i + 1) * 128], win_b.ap[:, c, 512:1024], [win_b, hT],
                   start=(c == 0), stop=(c == 7))
            for c in range(8):
                mm(bkv, bkv.ap[:, 0:512], hT.ap[:, c, i * 128:(i + 1) * 128], win_b.ap[:, c, 1024:1536], [win_b, hT],
                   start=(c == 0), stop=(c == 7))
            s_ = kvst[0]
            k.kvi += 1
            cp("act", s_.ap[:, 0, :], bkk.ap[:, 0:512], [bkk], [s_])
            cp("dve", s_.ap[:, 1, :], bkv.ap[:, 0:512], [bkv], [s_])
            st(kp_o[tile * 128:(tile + 1) * 128, :], s_.ap[:, 0, :], [s_])
            st(vp_o[tile * 128:(tile + 1) * 128, :], s_.ap[:, 1, :], [s_])
            if sample:
                si = tile - 16
                st(sq_s.ap[si, 1], s_.ap[:, 0, :], [s_], [sq_s])
                st(sq_s.ap[si, 2], s_.ap[:, 1, :], [s_], [sq_s])
                bkq = bank()
                for c in range(8):
                    mm(bkq, bkq.ap[:, 0:512], hT.ap[:, c, i * 128:(i + 1) * 128], win_b.ap[:, c, 0:512], [win_b, hT],
                       start=(c == 0), stop=(c == 7))
                cp("act", s_.ap[:, 0, :], bkq.ap[:, 0:512], [bkq], [s_])
                st(sq_s.ap[si, 0], s_.ap[:, 0, :], [s_], [sq_s])
            if not sample and 'vaug' not in SKIP:
                cp("dve", v_aug.ap[:, tile, :, 0:64], bkv.ap[:, 0:512].rearrange("p (h d) -> p h d", h=8), [bkv], [v_aug])


    def T(name, shape, dt):
        return P.sb(name, shape, dt)
    zs = T("zs", [128, 15, 128], F32)
    dtmp = T("dtmp", [128, 128], F32)
    lor_b = T("lor_b", [128, 128], BF16)
    sxg = T("sxg", [128, 128], BF16)
    sxg2 = T("sxg2", [32, 128], BF16)
    sigw = T("sigw", [128, 4, 128], F32)
    Aa = T("Aa", [128, 4, 128], F32)
    cl = T("cl", [128, 4, 128], F32)
    kkr = T("kkr", [128, 4, 128], F32)
    khh = T("khh", [128, 4, 128], F32)
    bb = T("bb", [128, 4, 128], F32)
    E1 = T("E1", [128, 4, 128], F32)
    E2 = T("E2", [128, 4, 128], F32)
    tm1 = T("tm1", [128, 4, 128], F32)
    gC = T("gC", [128, 4], F32)
    ones_f = T("ones_f", [128, 128], F32)
    memset("pool", ones_f, ones_f.ap, 1.0)
    sq_b = T("sq_b", [128, 4, 128], BF16)
    bt_b = T("bt_b", [128, 4, 128], BF16)
    ktl_b = T("ktl_b", [128, 4, 128], BF16)
    kr_b = T("kr_b", [128, 4, 2, 128], BF16)
    bh_b = T("bh_b", [128, 4, 128], BF16)
    kh_b = T("kh_b", [128, 4, 128], BF16)
    prod_b = T("prod_b", [128, 4, 128], BF16)
    vf_b = T("vf_b", [128, 4, 128], BF16)
    V_tm = T("V_tm", [128, 512], BF16)
    Bh_tm = T("Bh_tm", [128, 512], BF16)
    Kh_tm = T("Kh_tm", [128, 512], BF16)
    rkr = T("rkr", [128, 8], F32)
    Mb = [T("Mb%d" % i, [128, 2, 128], BF16) for i in range(2)]
    Mk = [T("Mk%d" % i, [128, 2, 128], BF16) for i in range(2)]
    Lt2 = [[T("Lt%d_%d" % (s_, i), [128, 128], BF16) for i in range(2)] for s_ in range(2)]
    Mt2 = [[T("Mt%d_%d" % (s_, i), [128, 128], BF16) for i in range(2)] for s_ in range(2)]
    Qt2 = [[T("Qt%d_%d" % (s_, i), [128, 128], BF16) for i in range(2)] for s_ in range(2)]
    W_b = [T("W_b%d" % i, [128, 64], BF16) for i in range(2)]
    U_b = T("U_b", [128, 8, 64], BF16)
    ST_f = [T("ST_f%d" % i, [128, 64], F32) for i in range(4)]
    ST_b = [T("ST_b%d" % i, [128, 64], BF16) for i in range(4)]
    ysb = T("ysb", [128, 8, 64], F32)
    yc = T("yc", [128, 8, 64], F32)
    ysq = T("ysq", [128, 8, 64], F32)
    gst = T("gst", [128, 4, 8], F32)
    rw_b = T("rw_b", [128, 512], BF16)
    mixT = T("mixT", [128, 8, 128], BF16)
    k.rot = 0

    def bc3(ap2, n):
        return ap2.unsqueeze(2).to_broadcast([ap2.shape[0], ap2.shape[1], n])

    def rwkv_prep(c0, C, single=False):
        zc = slice(1 + c0, 1 + c0 + C)
        zp = slice(c0, c0 + C)
        for ct in range(15):
            m = 128 if ct < 14 else 32
            tt("pool", dtmp.ap[0:m, 0:C], zraw.ap[0:m, ct, zp], zraw.ap[0:m, ct, zc], ALU.subtract, [zraw], [dtmp])
            stt(zs.ap[0:m, ct, 0:C], dtmp.ap[0:m, 0:C], muc.ap[0:m, ct:ct + 1], zraw.ap[0:m, ct, zc], ALU.mult, ALU.add,
                [dtmp, muc, zraw], [zs])
        R = zs.ap[:, 0:4, 0:C]
        Kf = zs.ap[:, 4:8, 0:C]
        Vf = zs.ap[:, 8:12, 0:C]
        act(lor_b.ap[0:64, 0:C], zs.ap[0:64, 12, 0:C], AF.Tanh, [zs], [lor_b])
        cp("pool", lor_b.ap[64:128, 0:C], zs.ap[64:128, 12, 0:C], [zs], [lor_b])
        act(sxg.ap[:, 0:C], zs.ap[:, 13, 0:C], AF.Sigmoid, [zs], [sxg])
        act(sxg2.ap[:, 0:C], zs.ap[0:32, 14, 0:C], AF.Sigmoid, [zs], [sxg2])
        bk = bank()
        bk2 = bank()
        for hp in range(4):
            mm(bk, bk.ap[:, hp * 128:hp * 128 + C], wa2_b.ap[0:64, hp * 128:(hp + 1) * 128], lor_b.ap[0:64, 0:C], [wa2_b, lor_b])
            mm(bk2, bk2.ap[:, hp * 128:hp * 128 + C], wa2_b.ap[64:128, hp * 128:(hp + 1) * 128], lor_b.ap[64:128, 0:C], [wa2_b, lor_b])
        for hp in range(4):
            act(sigw.ap[:, hp, 0:C], bk.ap[:, hp * 128:hp * 128 + C], AF.Sigmoid, [bk, prm["w0"]], [sigw], bias=prm["w0"].ap[:, hp:hp + 1])
            act(Aa.ap[:, hp, 0:C], bk2.ap[:, hp * 128:hp * 128 + C], AF.Sigmoid, [bk2, prm["a0"]], [Aa], bias=prm["a0"].ap[:, hp:hp + 1])
        ts("dve", sigw.ap[:, :, 0:C], sigw.ap[:, :, 0:C], -0.6065306597126334, None, ALU.mult, None, [sigw], [sigw])
        if single:
            cp("dve", cl.ap[:, :, 0:C], sigw.ap[:, :, 0:C], [sigw], [cl])
        for hp in range(0 if single else 4):
            P.op("dve", lambda e, hp=hp: e.tensor_tensor_scan(out=cl.ap[:, hp, 0:C], data0=ones_f.ap[:, 0:C], data1=sigw.ap[:, hp, 0:C],
                                                               initial=0.0, op0=ALU.mult, op1=ALU.add), reads=[ones_f, sigw], writes=[cl])
        tt("dve", kkr.ap[:, :, 0:C], Kf, bc3(prm["kk"].ap, C), ALU.mult, [zs, prm["kk"]], [kkr])
        tt("pool", sq_b.ap[:, :, 0:C], kkr.ap[:, :, 0:C], kkr.ap[:, :, 0:C], ALU.mult, [kkr], [sq_b])
        bk = bank()
        for hp in range(4):
            mm(bk, bk.ap[:, hp * 128:hp * 128 + C], bones_b.ap, sq_b.ap[:, hp, 0:C], [bones_b, sq_b])
        bkv = bk.ap[:, 0:512].rearrange("p (a b) -> p a b", a=4)[:, :, 0:C]
        ts("dve", tm1.ap[:, :, 0:C], bkv, 1e-24, None, ALU.max, None, [bk], [tm1])
        act(tm1.ap[:, :, 0:C], tm1.ap[:, :, 0:C], AF.Sqrt, [tm1], [tm1])
        P.op("dve", lambda e: e.reciprocal(out=tm1.ap[:, :, 0:C], in_=tm1.ap[:, :, 0:C]), reads=[tm1], writes=[tm1])
        tt("dve", kkr.ap[:, :, 0:C], kkr.ap[:, :, 0:C], tm1.ap[:, :, 0:C], ALU.mult, [kkr, tm1], [kkr])
        tt("pool", khh.ap[:, :, 0:C], Aa.ap[:, :, 0:C], bc3(prm["ka"].ap, C), ALU.mult, [Aa, prm["ka"]], [khh])
        tt("pool", khh.ap[:, :, 0:C], khh.ap[:, :, 0:C], bc3(omka.ap, C), ALU.add, [khh, omka], [khh])
        tt("pool", khh.ap[:, :, 0:C], khh.ap[:, :, 0:C], Kf, ALU.mult, [khh, zs], [khh])
        tt("dve", bb.ap[:, :, 0:C], kkr.ap[:, :, 0:C], Aa.ap[:, :, 0:C], ALU.mult, [kkr, Aa], [bb])
        act(E1.ap[:, :, 0:C], cl.ap[:, :, 0:C], AF.Exp, [cl], [E1])
        tt("dve", kr_b.ap[:, :, 1, 0:C], R, E1.ap[:, :, 0:C], ALU.mult, [zs, E1], [kr_b])
        tt("pool", tm1.ap[:, :, 0:C], cl.ap[:, :, 0:C], sigw.ap[:, :, 0:C], ALU.subtract, [cl, sigw], [tm1])
        act(E2.ap[:, :, 0:C], tm1.ap[:, :, 0:C], AF.Exp, [tm1], [E2])
        tt("dve", kr_b.ap[:, :, 0, 0:C], kkr.ap[:, :, 0:C], E2.ap[:, :, 0:C], ALU.mult, [kkr, E2], [kr_b])
        act(E1.ap[:, :, 0:C], cl.ap[:, :, 0:C], AF.Exp, [cl], [E1], scale=-1.0)
        tt("dve", bt_b.ap[:, :, 0:C], bb.ap[:, :, 0:C], E1.ap[:, :, 0:C], ALU.mult, [bb, E1], [bt_b])
        tt("pool", ktl_b.ap[:, :, 0:C], khh.ap[:, :, 0:C], E1.ap[:, :, 0:C], ALU.mult, [khh, E1], [ktl_b])
        if single:
            memset("pool", E2, E2.ap, 1.0)
        for hp in range(0 if single else 4):
            act(E2.ap[:, hp, 0:C], cl.ap[:, hp, 0:C], AF.Exp, [cl], [E2], scale=-1.0, bias=cl.ap[:, hp, C - 1:C])
        tt("dve", bh_b.ap[:, :, 0:C], bb.ap[:, :, 0:C], E2.ap[:, :, 0:C], ALU.mult, [bb, E2], [bh_b])
        tt("pool", kh_b.ap[:, :, 0:C], khh.ap[:, :, 0:C], E2.ap[:, :, 0:C], ALU.mult, [khh, E2], [kh_b])
        act(gC.ap, cl.ap[:, :, C - 1], AF.Exp, [cl], [gC])
        tt("pool", tm1.ap[:, :, 0:C], R, khh.ap[:, :, 0:C], ALU.mult, [zs, khh], [tm1])
        tt("pool", prod_b.ap[:, :, 0:C], tm1.ap[:, :, 0:C], bc3(prm["rk"].ap, C), ALU.mult, [tm1, prm["rk"]], [prod_b])
        cp("pool", vf_b.ap[:, :, 0:C], Vf, [zs], [vf_b])

    def rwkv_tm(C):
        bk = bank()
        for hp in range(4):
            mm(bk, bk.ap[0:C, 2 * hp:2 * hp + 2], prod_b.ap[:, hp, 0:C], ind_b.ap, [prod_b, ind_b])
        cp("dve", rkr.ap[0:C, :], bk.ap[0:C, 0:8], [bk], [rkr])
        for src_, dst_ in ((vf_b, V_tm), (bh_b, Bh_tm), (kh_b, Kh_tm)):
            bk = bank()
            bv = bfv(bk)
            for hp in range(4):
                tr(bk, bv[0:C, hp * 128:(hp + 1) * 128], src_.ap[:, hp, 0:C], ident_b.ap, [src_, ident_b])
            cp("act", dst_.ap[0:C, :], bv[0:C, 0:512], [bk], [dst_])

    def head_gen(hp, h2, C, rs, cs, yb, nlev):
        h = 2 * hp + h2
        ps_ = slice(h2 * 64, h2 * 64 + 64)
        mb = Mb[h2]
        mk = Mk[h2]
        b1 = bank()
        mm(b1, b1.ap[rs, 0:2 * 128].rearrange("p (a b) -> p a b", a=2)[:, :, 0:C], bt_b.ap[ps_, hp, cs], kr_b.ap[ps_, hp, :, cs], [bt_b, kr_b])
        tt("dve", mb.ap[rs, :, 0:C], b1.ap[rs, 0:256].rearrange("p (a b) -> p a b", a=2)[:, :, 0:C], masks.ap[0:C, 0:2, 0:C],
           ALU.mult, [b1, masks], [mb])
        b2 = bank()
        mm(b2, b2.ap[rs, 0:256].rearrange("p (a b) -> p a b", a=2)[:, :, 0:C], ktl_b.ap[ps_, hp, cs], kr_b.ap[ps_, hp, :, cs], [ktl_b, kr_b])
        tt("dve", mk.ap[rs, :, 0:C], b2.ap[rs, 0:256].rearrange("p (a b) -> p a b", a=2)[:, :, 0:C], masks.ap[0:C, 0:2, 0:C],
           ALU.mult, [b2, masks], [mk])
        cur = 0
        qcur = Qt2[h2][0]
        if C > 1:
            b3 = bank()
            mm(b3, b3.ap[rs, 0:C], kr_b.ap[ps_, hp, 0, cs], bt_b.ap[ps_, hp, cs], [kr_b, bt_b])
            lcur = Lt2[h2][0]
            mcur = Mt2[h2][0]
            tt("dve", lcur.ap[rs, 0:C], b3.ap[rs, 0:C], masks.ap[0:C, 2, 0:C], ALU.mult, [b3, masks], [lcur])
            cp("act", mcur.ap[rs, 0:C], mb.ap[rs, 0, 0:C], [mb], [mcur])
            tt("dve", qcur.ap[rs, 0:C], ident_f.ap[0:C, 0:C], mb.ap[rs, 0, 0:C], ALU.subtract, [ident_f, mb], [qcur])
            yield
            for lev in range(1, nlev + 1):
                nx = 1 - cur
                lnew = Lt2[h2][nx]
                mnew = Mt2[h2][nx]
                qnew = Qt2[h2][nx]
                bl = bank()
                mm(bl, bl.ap[rs, 0:C], mcur.ap[rs, 0:C], lcur.ap[rs, 0:C], [mcur, lcur])
                if lev < nlev:
                    mm(bl, bl.ap[rs, 128:128 + C], lcur.ap[rs, 0:C], mcur.ap[rs, 0:C], [mcur, lcur])
                cp("act", lnew.ap[rs, 0:C], bl.ap[rs, 0:C], [bl], [lnew])
                if lev < nlev:
                    cp("act", mnew.ap[rs, 0:C], bl.ap[rs, 128:128 + C], [bl], [mnew])
                yield
                mm(bl, bl.ap[rs, 256:256 + C], lnew.ap[rs, 0:C], qcur.ap[rs, 0:C], [lnew, qcur])
                tt("dve", qnew.ap[rs, 0:C], bl.ap[rs, 256:256 + C], qcur.ap[rs, 0:C], ALU.add, [bl, qcur], [qnew])
                lcur, mcur, qcur = lnew, mnew, qnew
                cur = nx
                yield
        else:
            cp("dve", qcur.ap[rs, 0:C], ident_f.ap[0:1, 0:1], [ident_f], [qcur])
        wb = W_b[h2]
        bw = bank()
        mm(bw, bw.ap[rs, 0:64], kr_b.ap[ps_, hp, 0, cs], ST_b[hp].ap[ps_, :], [kr_b, ST_b[hp]], start=True, stop=False)
        mm(bw, bw.ap[rs, 0:64], mk.ap[rs, 0, 0:C], V_tm.ap[rs, h * 64:(h + 1) * 64], [mk, V_tm], start=False, stop=True)
        cp("act", wb.ap[rs, :], bw.ap[rs, 0:64], [bw], [wb])
        yield
        bu = bank()
        mm(bu, bu.ap[rs, 0:64], qcur.ap[rs, 0:C], wb.ap[rs, :], [qcur, wb])
        ts("dve", U_b.ap[rs, h, :], bu.ap[rs, 0:64], -1.0, None, ALU.mult, None, [bu], [U_b])
        yield
        yo = yb.ap[rs, h * 64:(h + 1) * 64]
        mm(yb, yo, kr_b.ap[ps_, hp, 1, cs], ST_b[hp].ap[ps_, :], [kr_b, ST_b[hp]], start=True, stop=False)
        mm(yb, yo, mb.ap[rs, 1, 0:C], U_b.ap[rs, h, :], [mb, U_b], start=False, stop=False)
        mm(yb, yo, mk.ap[rs, 1, 0:C], V_tm.ap[rs, h * 64:(h + 1) * 64], [mk, V_tm], start=False, stop=True)

    def rwkv_chunk_g(C, yb, r0=0, c0=0):
        rs = slice(r0, r0 + C)
        cs = slice(c0, c0 + C)
        nlev = 0
        while (1 << nlev) < C:
            nlev += 1
        for hp in range(4):
            gens = [head_gen(hp, 0, C, rs, cs, yb, nlev), head_gen(hp, 1, C, rs, cs, yb, nlev)]
            while gens:
                for g in list(gens):
                    try:
                        next(g)
                    except StopIteration:
                        gens.remove(g)
                yield
            bs = bank()
            mm(bs, bs.ap[:, 0:128], Bh_tm.ap[rs, hp * 128:(hp + 1) * 128], U_b.ap[rs, 2 * hp:2 * hp + 2, :], [Bh_tm, U_b],
               start=True, stop=False)
            mm(bs, bs.ap[:, 0:128], Kh_tm.ap[rs, hp * 128:(hp + 1) * 128], V_tm.ap[rs, hp * 128:(hp + 1) * 128], [Kh_tm, V_tm],
               start=False, stop=True)
            for h2 in range(2):
                ps_ = slice(h2 * 64, h2 * 64 + 64)
                stt(ST_f[hp].ap[ps_, :], ST_f[hp].ap[ps_, :], gC.ap[ps_, hp:hp + 1], bs.ap[ps_, h2 * 64:(h2 + 1) * 64],
                    ALU.mult, ALU.add, [ST_f[hp], gC, bs], [ST_f[hp]])
            cp("pool", ST_b[hp].ap, ST_f[hp].ap, [ST_f[hp]], [ST_b[hp]])
            yield

    def rwkv_post(yb, C, r0, dst_cols):
        rs = slice(r0, r0 + C)
        y3 = yb.ap[rs, 0:512].rearrange("p (h d) -> p h d", h=8)
        cp("act", ysb.ap[rs], y3, [yb], [ysb])
        red(gst.ap[rs, 0, :], ysb.ap[rs], ALU.add, [ysb], [gst])
        ts("dve", gst.ap[rs, 0, :], gst.ap[rs, 0, :], 1.0 / 64, None, ALU.mult, None, [gst], [gst])
        tt("dve", yc.ap[rs], ysb.ap[rs], bc3(gst.ap[rs, 0, :], 64), ALU.subtract, [ysb, gst], [yc])
        tt("pool", ysq.ap[rs], yc.ap[rs], yc.ap[rs], ALU.mult, [yc], [ysq])
        red(gst.ap[rs, 1, :], ysq.ap[rs], ALU.add, [ysq], [gst])
        ts("dve", gst.ap[rs, 1, :], gst.ap[rs, 1, :], 1.0 / 64, 64e-5, ALU.mult, ALU.add, [gst], [gst])
        act(gst.ap[rs, 2, :], gst.ap[rs, 1, :], AF.Sqrt, [gst], [gst])
        P.op("dve", lambda e: e.reciprocal(out=gst.ap[rs, 3, :], in_=gst.ap[rs, 2, :]), reads=[gst], writes=[gst])
        tt("dve", yc.ap[rs], yc.ap[rs], bc3(gst.ap[rs, 3, :], 64), ALU.mult, [yc, gst], [yc])
        ycf = yc.ap[rs].rearrange("p h d -> p (h d)")
        tt("pool", ycf, ycf, lgb.ap[rs, :], ALU.mult, [yc, lgb], [yc])
        tt("pool", ycf, ycf, lbb.ap[rs, :], ALU.add, [yc, lbb], [yc])
        tt("dve", ysq.ap[rs], V_tm.ap[rs, :].rearrange("p (h d) -> p h d", h=8), bc3(rkr.ap[rs, :], 64), ALU.mult, [V_tm, rkr], [ysq])
        tt("dve", yc.ap[rs], yc.ap[rs], ysq.ap[rs], ALU.add, [yc, ysq], [yc])
        bg = bank()
        mm(bg, bg.ap[rs, 0:512], sxg.ap[:, dst_cols] if False else sxg.ap[:, r0:r0 + C], g2a_b.ap, [sxg, g2a_b], start=True, stop=False)
        mm(bg, bg.ap[rs, 0:512], sxg2.ap[:, r0:r0 + C], g2b_b.ap, [sxg2, g2b_b], start=False, stop=True)
        tt("dve", rw_b.ap[rs, :], ycf, bg.ap[rs, 0:512], ALU.mult, [yc, bg], [rw_b])

    def rw_to_T(dst_cols):
        bk = bank()
        bv = bfv(bk)
        for hp in range(4):
            tr(bk, bv[:, hp * 128:(hp + 1) * 128], rw_b.ap[:, hp * 128:(hp + 1) * 128], ident_b.ap, [rw_b, ident_b])
        cp("act", mixT.ap[:, 4:8, dst_cols], bv[:, 0:512].rearrange("p (c t) -> p c t", c=4), [bk], [mixT])

    def st_out(dst):
        for hp in range(4):
            bk = bank()
            tr(bk, bk.ap[0:64, 0:128], ST_f[hp].ap, ident_f.ap, [ST_f[hp], ident_f])
            cp("dve", ysb.ap[0:64, 2 * hp:2 * hp + 2, :], bk.ap[0:64, 0:128].rearrange("p (h j) -> p h j", h=2), [bk], [ysb])
        st(dst.rearrange("h i j -> i h j"), ysb.ap[0:64, :, :], [ysb])

    def phase1_rwkv(gi, t0, nt):
        sample = (t0 >= 16)
        if not sample:
            if gi == 0:
                for hp in range(4):
                    memset("pool", ST_f[hp], ST_f[hp].ap, 0.0)
                    memset("pool", ST_b[hp], ST_b[hp].ap, 0.0)
            for i in range(nt):
                rwkv_prep(i * 128, 128)
                rwkv_tm(128)
                yield
                yb = bank(reserve=True)
                yield from rwkv_chunk_g(128, yb)
                rwkv_post(yb, 128, 0, None)
                release(yb)
                rw_to_T(slice(i * 128, (i + 1) * 128))
            if t0 + nt == 16:
                st_out(wkvp_o)
        else:
            rwkv_prep(0, 128, single=True)
            rwkv_tm(128)
            memset("pool", rw_b, rw_b.ap, 0.0)
            yb = bank(reserve=True)
            for b, r in SMAP[t0]:
                for hp in range(4):
                    ld(ysq, ysq.ap[0:64, 0:2, :], swkv_d[b, 2 * hp:2 * hp + 2].rearrange("h i j -> i h j"))
                    bk = bank()
                    tr(bk, bk.ap[:, 0:64], ysq.ap[0:64, 0:2, :].rearrange("p h j -> p (h j)"), ident_f.ap[0:64, 0:64], [ysq, ident_f])
                    cp("dve", ST_f[hp].ap, bk.ap[:, 0:64], [bk], [ST_f[hp]])
                    cp("pool", ST_b[hp].ap, ST_f[hp].ap, [ST_f[hp]], [ST_b[hp]])
                act(gC.ap, cl.ap[:, :, r], AF.Exp, [cl], [gC])
                for _ in rwkv_chunk_g(1, yb, r0=r, c0=r):
                    pass
                st_out(wkvs_o[b])
            for b, r in SMAP[t0]:
                rwkv_post(yb, 1, r, None)
            release(yb)
            rw_to_T(slice(0, 128))


    Bd_b = T("Bd_b", [128, 8, 128], BF16)
    Bo_b = T("Bo_b", [128, 8, 128], BF16)
    E8 = T("E8", [8, 8, 128], BF16)
    g1 = T("g1", [128, 8, 8], F32)
    g2_ = T("g2_", [128, 8, 8], F32)
    eqt = T("eqt", [128, 8, 8], F32)
    mx = T("mx", [128, 8], F32)
    seln = T("seln", [128, 8, 8], BF16)
    selT = T("selT", [8, 8, 128], BF16)
    PT_b = [T("PT_b%d" % i, [128, 4, 128], BF16) for i in range(2)]
    att_b = T("att_b", [128, 8, 64], BF16)
    rcp = T("rcp", [128, 8], F32)
    k.pti = 0

    def attn_setup():
        ld(rb_sb, rb_sb.ap, rb_d)
        ld(oh_sb, oh_sb.ap, c_oh)
        ld(negf_sb, negf_sb.ap, c_negf)
        cp("dve", rbB.ap, bc3(rb_sb.ap, 128), [rb_sb], [rbB])
        cp("dve", E8.ap, bc3(ident_f.ap[0:8, 0:8], 128), [ident_f], [E8])
        for h in range(8):
            bk = bank()
            mm(bk, bk.ap[:, 0:384], rbB.ap[:, h, :], oh_sb.ap, [rbB, oh_sb])
            ts("dve", Frow.ap, bk.ap[:, 0:384], b31.ap[:, h:h + 1], 8.0, ALU.subtract, ALU.mult, [bk, b31], [Frow])
            tt("dve", Frow.ap, Frow.ap, negf_sb.ap, ALU.add, [Frow, negf_sb], [Frow])
            st(bias_s.ap[h].rearrange("(p c) -> p c", c=384), Frow.ap, [Frow], [bias_s])
            for off, dst in ((127, Bd_b), (255, Bo_b)):
                src_ap = bass.AP(bias_s.ap.tensor, h * 128 * 384 + off, [[383, 128], [1, 128]])
                ld(Bst, Bst.ap, src_ap, reads=[bias_s])
                cp("dve", dst.ap[:, h, :], Bst.ap, [Bst], [dst])

    def attn_tile(qt, qc0, dst_cols):
        n_own = qt // 2
        import os
        need_sel = n_own >= 4 and 'nosel' not in os.environ.get('DBG_SKIP', '')
        qcs = slice(qc0, qc0 + 128)
        if need_sel and 'selB' not in os.environ.get('DBG_SKIP', ''):
            bk = bank()
            for h in range(8):
                ps_ = slice((h % 2) * 64, (h % 2) * 64 + 64)
                for n4 in range(2):
                    mm(bk, bk.ap[:, h * 8 + n4 * 4:h * 8 + n4 * 4 + 4], qT_g.ap[ps_, h // 2, qcs], kmT_b.ap[ps_, h // 2, n4 * 4:n4 * 4 + 4],
                       [qT_g, kmT_b])
            tt("dve", g1.ap.rearrange("p h n -> p (h n)"), bk.ap[:, 0:64], nio.ap[:, n_own - 4, :], ALU.add, [bk, nio], [g1])
            red(mx.ap, g1.ap, ALU.max, [g1], [mx])
            tt("dve", eqt.ap, g1.ap, bc3(mx.ap, 8), ALU.is_ge, [g1, mx], [eqt])
            stt(g2_.ap, eqt.ap, -3e30, g1.ap, ALU.mult, ALU.add, [eqt, g1], [g2_])
            red(mx.ap, g2_.ap, ALU.max, [g2_], [mx])
            tt("dve", eqt.ap, g2_.ap, bc3(mx.ap, 8), ALU.is_ge, [g2_, mx], [eqt])
            stt(g2_.ap, eqt.ap, -3e30, g2_.ap, ALU.mult, ALU.add, [eqt, g2_], [g2_])
            red(mx.ap, g2_.ap, ALU.max, [g2_], [mx])
            tt("dve", eqt.ap, g1.ap, bc3(mx.ap, 8), ALU.is_ge, [g1, mx], [eqt])
            ts("dve", seln.ap, eqt.ap, -NEGB, NEGB, ALU.mult, ALU.add, [eqt], [seln])
            bk = bank()
            bv = bfv(bk)
            for h in range(0 if 'selC' in os.environ.get('DBG_SKIP', '') else 8):
                tr(bk, bv[0:8, h * 128:(h + 1) * 128], seln.ap[:, h, :], ident_b.ap, [seln, ident_b])
            if 'selC' not in os.environ.get('DBG_SKIP', '') and 'selD' not in os.environ.get('DBG_SKIP', ''):
                cp("act", selT.ap, bv[0:8, 0:1024].rearrange("p (h q) -> p h q", h=8), [bk], [selT])
        OA = bank(reserve=True)
        OB = bank(reserve=True)
        for h in range(8):
            hp = h // 2
            ps_ = slice((h % 2) * 64, (h % 2) * 64 + 64)
            ob = OA if h < 4 else OB
            oreg = ob.ap[:, (h % 4) * 65:(h % 4) * 65 + 65]
            for kc0 in range(0, qt + 1, 4):
                kts = list(range(kc0, min(kc0 + 4, qt + 1)))
                sb_ = bank()
                for j, kt in enumerate(kts):
                    extra = []
                    if kt == qt:
                        extra.append((ident_b.ap, Bd_b.ap[:, h, :], [ident_b, Bd_b]))
                    elif kt == qt - 1:
                        extra.append((ident_b.ap, Bo_b.ap[:, h, :], [ident_b, Bo_b]))
                    if need_sel and kt // 2 < n_own and 'selA' not in os.environ.get('DBG_SKIP', ''):
                        extra.append((E8.ap[:, kt // 2, :], selT.ap[:, h, :], [E8, selT]))
                    so = sb_.ap[:, j * 128:(j + 1) * 128]
                    mm(sb_, so, kT_all.ap[ps_, hp, kt * 128:(kt + 1) * 128], qT_g.ap[ps_, hp, qcs], [kT_all, qT_g],
                       start=True, stop=(len(extra) == 0))
                    for ei, (l_, r_, rd_) in enumerate(extra):
                        mm(sb_, so, l_, r_, rd_, start=False, stop=(ei == len(extra) - 1))
                pt = PT_b[k.pti % 2]
                k.pti += 1
                nj = len(kts)
                act(pt.ap[:, 0:nj, :], sb_.ap[:, 0:nj * 128].rearrange("p (j q) -> p j q", j=nj), AF.Exp, [sb_, b31], [pt],
                    bias=b31.ap[:, h:h + 1], scale=0.125)
                for j, kt in enumerate(kts):
                    mm(ob, oreg, pt.ap[:, j, :], v_aug.ap[:, kt, h, 0:65], [pt, v_aug], start=(kt == 0), stop=(kt == qt))
                yield
        for ob, h0 in ((OA, 0), (OB, 4)):
            o3 = ob.ap[:, 0:260].rearrange("p (h d) -> p h d", h=4)
            P.op("dve", lambda e, o3=o3, h0=h0: e.reciprocal(out=rcp.ap[:, h0:h0 + 4], in_=o3[:, :, 64]), reads=[ob], writes=[rcp])
            tt("dve", att_b.ap[:, h0:h0 + 4, :], o3[:, :, 0:64], bc3(rcp.ap[:, h0:h0 + 4], 64), ALU.mult, [ob, rcp], [att_b])
        release(OA)
        release(OB)
        bk = bank()
        bv = bfv(bk)
        af = att_b.ap.rearrange("p h d -> p (h d)")
        for c in range(4):
            tr(bk, bv[:, c * 128:(c + 1) * 128], af[:, c * 128:(c + 1) * 128], ident_b.ap, [att_b, ident_b])
        cp("act", mixT.ap[:, 0:4, dst_cols], bv[:, 0:512].rearrange("p (c t) -> p c t", c=4), [bk], [mixT])

    x1t = Buf(zs.ap[:, 0:8, :].rearrange("p a b -> p (a b)"), "x1t")
    x1t_buf = zs

    def out_proj(tile, cols):
        xb = xt[0]
        ld(xb, xb.ap, x_d[tile * 128:(tile + 1) * 128, :])
        for hf in range(2):
            bk = bank()
            for c in range(8):
                mm(bk, bk.ap[:, 0:512], mixT.ap[:, c, cols], wout_b.ap[:, c, hf * 512:(hf + 1) * 512], [mixT, wout_b],
                   start=(c == 0), stop=(c == 7))
            tt("dve", x1t.ap[:, hf * 512:(hf + 1) * 512], bk.ap[:, 0:512], xb.ap[:, hf * 512:(hf + 1) * 512], ALU.add, [bk, xb], [x1t_buf])
        st(x1_s.ap[tile * 128:(tile + 1) * 128, :], x1t.ap, [x1t_buf], [x1_s])

    def phase1_rest(gi, t0, nt):
        sample = (t0 >= 16)
        for i in range(nt):
            cols = slice(i * 128, (i + 1) * 128)
            if not sample:
                out_proj(t0 + i, cols)
            else:
                st(smix_s.ap[t0 - 16], mixT.ap[:, 4:8, :], [mixT], [smix_s])

    if stage >= 3:
        P.flush()
        rb_sb = T("rb_sb", [32, 8], F32)
        oh_sb = Buf(cl.ap[0:32, 0:3, :].rearrange("p a b -> p (a b)"), "oh_sb")
        negf_sb = Buf(zs.ap[:, 3:6, :].rearrange("p a b -> p (a b)"), "negf_sb")
        rbB = Buf(zraw.ap[0:32, 0:8, 0:128], "rbB")
        Frow = Buf(zs.ap[:, 0:3, :].rearrange("p a b -> p (a b)"), "Frow")
        Bst = Buf(zs.ap[:, 6, :], "Bst")
        attn_setup()
        P.flush()
        memset("pool", zraw, zraw.ap, 0.0)
    if stage >= 1:
        for gi, (t0, nt) in enumerate(groups):
            phase1_proj(gi, t0, nt)
            gens = []
            if stage >= 2:
                gens.append(phase1_rwkv(gi, t0, nt))
            if stage >= 3 and t0 < 16:
                assert nt == 1
                gens.append(attn_tile(t0, 0, slice(0, 128)))
            while gens:
                for g in list(gens):
                    try:
                        next(g)
                    except StopIteration:
                        gens.remove(g)
            if stage >= 3:
                phase1_rest(gi, t0, nt)


    P.flush()
    ph1.close()
    ph1b = ExitStack()
    P.es = ph1b
    if stage >= 5:
        pt_i = T("pt_i", [128, 64], I32)
        iota_i = T("iota_i", [128, 1], I32)
        idx_i = T("idx_i", [128, 64], I32)
        kpg = [T("kpg%d" % i, [128, 512], F32) for i in range(3)]
        SC = T("SC", [128, 64, 8], F32)
        PP = T("PP", [128, 64, 8], F32)
        biasfull = T("biasfull", [128, 64, 8], F32)
        selB = T("selB", [128, 8, 32], F32)
        qb = T("qb", [128, 512], F32)
        tmpk = T("tmpk", [128, 512], F32)
        Er = T("Er", [128, 128], F32)
        Zsel = T("Zsel", [128, 63], F32)
        rb2 = T("rb2", [32, 8], F32)
        ohs_sb = T("ohs_sb", [32, 128], F32)
        b0b = T("b0b", [128, 8], F32)
        bm8 = T("bm8", [8, 512], F32)
        ones8 = T("ones8", [8, 128], F32)
        gate_s = T("gate_s", [32, 8], F32)
        gT = T("gT", [8, 32], F32)
        gT2 = T("gT2", [8, 32], F32)
        eq8 = T("eq8", [8, 32], F32)
        mx8 = T("mx8", [8, 4], F32)
        Rexp = T("Rexp", [8, 8, 32], F32)
        pown = T("pown", [128, 8], F32)
        ones_c = T("ones_c", [128, 1], F32)
        Osb = T("Osb", [8, 512], F32)
        rden = T("rden", [8, 2], F32)
        xtb = T("xtb", [128, 1024], F32)
        smix = T("smix", [128, 8, 2, 128], BF16)
        sqkv = T("sqkv", [128, 2, 3, 512], F32)
        x1tb = T("x1tb", [128, 1024], F32)
        k.kpi = 0

        P.op("pool", lambda e: e.iota(iota_i.ap, [[0, 1]], base=0, channel_multiplier=1), reads=[], writes=[iota_i])
        memset("pool", Zsel, Zsel.ap, 0.0)
        memset("pool", Zsel, Zsel.ap[:, 31:32], 1.0 / 256)
        memset("pool", ones8, ones8.ap, 1.0)
        memset("pool", ones_c, ones_c.ap, 1.0)
        ld(rb2, rb2.ap, rb_d)
        ld(ohs_sb, ohs_sb.ap, c_ohs)
        ld(b0b, b0b.ap, rb_d[0, :].partition_broadcast(128))
        ld(bm8, bm8.ap, c_bm8)
        cp("dve", biasfull.ap, b31.ap.unsqueeze(1).to_broadcast([128, 64, 8]), [b31], [biasfull])
        bk = bank()
        mm(bk, bk.ap[:, 0:8], ohs_sb.ap, rb2.ap, [ohs_sb, rb2])
        cp("dve", biasfull.ap[:, 63, :], bk.ap[:, 0:8], [bk], [biasfull])

        def sample_attn(si, b, r):
            ld(pt_i, pt_i.ap, pt_d[b].partition_broadcast(128))
            stt(idx_i.ap, pt_i.ap, 128.0, iota_i.ap.to_broadcast([128, 64]), ALU.mult, ALU.add, [pt_i, iota_i], [idx_i])
            cp("dve", Er.ap, ident_f.ap[:, r:r + 1].to_broadcast([128, 128]), [ident_f], [Er])
            bk = bank()
            mm(bk, bk.ap[:, 0:512], Er.ap, sqkv.ap[:, si, 0, :], [Er, sqkv])
            cp("act", qb.ap, bk.ap[:, 0:512], [bk], [qb])
            KM = bank(reserve=True)
            for pg in range(64):
                kb = kpg[k.kpi % 3]
                k.kpi += 1
                P.dma("pool", lambda e, kb=kb, pg=pg: e.indirect_dma_start(
                    out=kb.ap, out_offset=None, in_=ck_d,
                    in_offset=bass.IndirectOffsetOnAxis(ap=idx_i.ap[:, pg:pg + 1], axis=0)), reads=[idx_i], writes=[kb])
                n = pg // 2
                mm(KM, KM.ap[0:32, 0:512], Zsel.ap[:, 31 - n:63 - n], kb.ap, [Zsel, kb], start=(pg == 0), stop=(pg == 63))
                tt("pool", tmpk.ap, kb.ap, qb.ap, ALU.mult, [kb, qb], [tmpk])
                red(SC.ap[:, pg, :], tmpk.ap.rearrange("p (h d) -> p h d", h=8), ALU.add, [tmpk], [SC])
            tt("dve", tmpk.ap[0:32, :], KM.ap[0:32, 0:512], qb.ap[0:32, :], ALU.mult, [KM, qb], [tmpk])
            release(KM)
            red(gate_s.ap, tmpk.ap[0:32, :].rearrange("p (h d) -> p h d", h=8), ALU.add, [tmpk], [gate_s])
            bk = bank()
            tr(bk, bk.ap[0:8, 0:32], gate_s.ap, ident_f.ap[0:32, 0:32], [gate_s, ident_f])
            cp("dve", gT.ap, bk.ap[0:8, 0:32], [bk], [gT])
            red(mx8.ap[:, 0:1], gT.ap, ALU.max, [gT], [mx8])
            tt("dve", eq8.ap, gT.ap, mx8.ap[:, 0:1].to_broadcast([8, 32]), ALU.is_ge, [gT, mx8], [eq8])
            stt(gT2.ap, eq8.ap, -3e30, gT.ap, ALU.mult, ALU.add, [eq8, gT], [gT2])
            red(mx8.ap[:, 1:2], gT2.ap, ALU.max, [gT2], [mx8])
            tt("dve", eq8.ap, gT2.ap, mx8.ap[:, 1:2].to_broadcast([8, 32]), ALU.is_ge, [gT2, mx8], [eq8])
            stt(gT2.ap, eq8.ap, -3e30, gT2.ap, ALU.mult, ALU.add, [eq8, gT2], [gT2])
            red(mx8.ap[:, 2:3], gT2.ap, ALU.max, [gT2], [mx8])
            tt("dve", eq8.ap, gT.ap, mx8.ap[:, 2:3].to_broadcast([8, 32]), ALU.is_ge, [gT, mx8], [eq8])
            tt("dve", Rexp.ap, eq8.ap.unsqueeze(1).to_broadcast([8, 8, 32]),
               bm8.ap.rearrange("p (h d) -> p h d", h=8)[:, :, 0:32], ALU.mult, [eq8, bm8], [Rexp])
            bk = bank()
            mm(bk, bk.ap[:, 0:256], ones8.ap, Rexp.ap.rearrange("p h n -> p (h n)"), [ones8, Rexp])
            cp("dve", selB.ap, bk.ap[:, 0:256].rearrange("p (h n) -> p h n", h=8), [bk], [selB])
            stt(PP.ap, SC.ap, 0.125, biasfull.ap, ALU.mult, ALU.add, [SC, biasfull], [PP])
            act(PP.ap, PP.ap, AF.Exp, [PP], [PP])
            tt("dve", PP.ap.rearrange("p (n t) h -> p n t h", t=2), PP.ap.rearrange("p (n t) h -> p n t h", t=2),
               selB.ap.rearrange("p h n -> p n h").unsqueeze(2).to_broadcast([128, 32, 2, 8]), ALU.mult, [PP, selB], [PP])
            tt("dve", tmpk.ap, sqkv.ap[:, si, 0, :], sqkv.ap[:, si, 1, :], ALU.mult, [sqkv], [tmpk])
            red(pown.ap, tmpk.ap.rearrange("p (h d) -> p h d", h=8), ALU.add, [tmpk], [pown])
            stt(pown.ap, pown.ap, 0.125, b0b.ap, ALU.mult, ALU.add, [pown, b0b], [pown])
            act(pown.ap, pown.ap, AF.Exp, [pown], [pown])
            ts("dve", pown.ap, pown.ap, ident_f.ap[:, r:r + 1], None, ALU.mult, None, [pown, ident_f], [pown])
            OB_ = bank(reserve=True)
            DB_ = bank(reserve=True)
            for pg in range(64):
                vb = kpg[k.kpi % 3]
                k.kpi += 1
                P.dma("pool", lambda e, vb=vb, pg=pg: e.indirect_dma_start(
                    out=vb.ap, out_offset=None, in_=cv_d,
                    in_offset=bass.IndirectOffsetOnAxis(ap=idx_i.ap[:, pg:pg + 1], axis=0)), reads=[idx_i], writes=[vb])
                mm(OB_, OB_.ap[0:8, 0:512], PP.ap[:, pg, :], vb.ap, [PP, vb], start=(pg == 0), stop=False)
                mm(DB_, DB_.ap[0:8, 0:1], PP.ap[:, pg, :], ones_c.ap, [PP, ones_c], start=(pg == 0), stop=False)
            mm(OB_, OB_.ap[0:8, 0:512], pown.ap, sqkv.ap[:, si, 2, :], [pown, sqkv], start=False, stop=True)
            mm(DB_, DB_.ap[0:8, 0:1], pown.ap, ones_c.ap, [pown, ones_c], start=False, stop=True)
            tt("dve", Osb.ap, OB_.ap[0:8, 0:512], bm8.ap, ALU.mult, [OB_, bm8], [Osb])
            P.op("dve", lambda e: e.reciprocal(out=rden.ap[:, 0:1], in_=DB_.ap[0:8, 0:1]), reads=[DB_], writes=[rden])
            release(OB_)
            release(DB_)
            bk = bank()
            for hp in range(4):
                mm(bk, bk.ap[:, hp:hp + 1], Osb.ap[:, hp * 128:(hp + 1) * 128], rden.ap[:, 0:1], [Osb, rden])
            cp("dve", smix.ap[:, 0:4, si, r], bk.ap[:, 0:4], [bk], [smix])

        for si in range(2):
            tile = 16 + si
            if tile not in [t0 for (t0, nt) in groups]:
                continue
            memset("pool", smix, smix.ap[:, 0:4, si, :], 0.0)
            ld(smix, smix.ap[:, 4:8, si, :], smix_s.ap[si], reads=[smix_s])
            for j3 in range(3):
                ld(sqkv, sqkv.ap[:, si, j3, :], sq_s.ap[si, j3], reads=[sq_s])
            for b, r in SMAP[tile]:
                sample_attn(si, b, r)
            ld(xtb, xtb.ap, x_d[tile * 128:(tile + 1) * 128, :])
            for hf in range(2):
                bk = bank()
                for c in range(8):
                    mm(bk, bk.ap[:, 0:512], smix.ap[:, c, si, :], wout_b.ap[:, c, hf * 512:(hf + 1) * 512], [smix, wout_b],
                       start=(c == 0), stop=(c == 7))
                tt("dve", x1tb.ap[:, hf * 512:(hf + 1) * 512], bk.ap[:, 0:512], xtb.ap[:, hf * 512:(hf + 1) * 512], ALU.add, [bk, xtb], [x1tb])
            st(x1_s.ap[tile * 128:(tile + 1) * 128, :], x1tb.ap, [x1tb], [x1_s])
    P.flush()
    ph1b.close()
    phS.close()

    ph2 = ExitStack()
    P.es = ph2
    ntl = [t0 for (t0, nt) in groups]
    NTT = 18
    h2T = T("h2T", [128, 8, NTT * 128], BF16)
    acc = T("acc", [128, NTT, 1024], F32)
    comb = T("comb", [128, NTT, 16], F32)
    fgb = T("fgb", [128, 1024], F32)
    ld(fgb, fgb.ap, fg_d.partition_broadcast(128))
    wpleg_b = T("wpleg_b", [128, 8, 1024], BF16)
    wple_b = T("wple_b", [128, 2, 1024], BF16)
    wr_b = T("wr_b", [128, 8, 20], BF16)
    wr_f = T("wr_f", [128, 8, 20], F32)
    brb = T("brb", [128, 20], F32)
    stg4 = T("stg4", [128, 2, 1024], F32)
    stg2 = Buf(stg4.ap.rearrange("p a b -> p (a b)").rearrange("p (c f) -> p c f", c=8), "stg2")
    weg_b = [T("weg_b%d" % i, [128, 8, 256], BF16) for i in range(2)]
    weu_b = [T("weu_b%d" % i, [128, 8, 256], BF16) for i in range(2)]
    wed_b = [T("wed_b%d" % i, [128, 2, 1024], BF16) for i in range(2)]
    actT = [T("actT%d" % i, [128, 2, 512], BF16) for i in range(2)]
    sil = T("sil", [128, 2, 512], F32)
    xs_b2 = T("xs_b2", [128, 1024], BF16)
    sqj2 = T("sqj2", [128, 1024], BF16)
    nst2 = T("nst2", [128, 8], F32)
    lg = T("lg", [128, 20], F32)
    rt = T("rt", [128, 16, 4], F32)
    rtm = T("rtm", [128, 16], F32)
    x2T = T("x2T", [128, 8, 128], BF16)
    pt_f = T("pt_f", [128, 256], F32)
    pt_b = T("pt_b", [128, 256], BF16)
    pT = T("pT", [128, 2, 128], BF16)
    sgt = T("sgt", [128, 1024], F32)
    yt = T("yt", [128, 1024], F32)

    for c in range(8):
        ld(stg4, stg4.ap[:, 0, :], wpleg_d[c * 128:(c + 1) * 128, :])
        cp("pool", wpleg_b.ap[:, c, :], stg4.ap[:, 0, :], [stg4], [wpleg_b])
    for c in range(2):
        ld(stg4, stg4.ap[:, 1, :], wple_d[c * 128:(c + 1) * 128, :])
        cp("pool", wple_b.ap[:, c, :], stg4.ap[:, 1, :], [stg4], [wple_b])
    ld(wr_f, wr_f.ap[:, :, 0:4], wrg_d.rearrange("(c p) g -> p c g", p=128), nc_ok=True)
    ld(wr_f, wr_f.ap[:, :, 4:20], wre_d.rearrange("(c p) g -> p c g", p=128), nc_ok=True)
    cp("dve", wr_b.ap, wr_f.ap, [wr_f], [wr_b])
    ld(brb, brb.ap[:, 0:4], brg_d.partition_broadcast(128))
    ld(brb, brb.ap[:, 4:20], bre_d.partition_broadcast(128))

    def rms2(src_ap, src_buf, gcol, dstT, dst_cols):
        act(sqj2.ap, src_ap, AF.Square, [src_buf], [sqj2, nst2], accum=nst2.ap[:, 0:1])
        ts("dve", nst2.ap[:, 1:2], nst2.ap[:, 0:1], 1.0 / 1024, 1e-6, ALU.mult, ALU.add, [nst2], [nst2])
        act(nst2.ap[:, 2:3], nst2.ap[:, 1:2], AF.Sqrt, [nst2], [nst2])
        P.op("dve", lambda e: e.reciprocal(out=nst2.ap[:, 3:4], in_=nst2.ap[:, 2:3]), reads=[nst2], writes=[nst2])

    def to_T(src_ap, src_buf, dstT, dst_cols, gcol=None, scale_col=None):
        if scale_col is not None:
            ts("dve", xs_b2.ap, src_ap, scale_col, None, ALU.mult, None, [src_buf, nst2], [xs_b2])
        else:
            cp("dve", xs_b2.ap, src_ap, [src_buf], [xs_b2])
        bk = bank()
        bv = bfv(bk)
        for c in range(8):
            tr(bk, bv[:, c * 128:(c + 1) * 128], xs_b2.ap[:, c * 128:(c + 1) * 128], ident_b.ap, [xs_b2, ident_b])
        if gcol is not None:
            tt("dve", dstT.ap[:, :, dst_cols], bv[:, 0:1024].rearrange("p (c t) -> p c t", c=8),
               gcol.ap.unsqueeze(2).to_broadcast([128, 8, 128]), ALU.mult, [bk, gcol], [dstT])
        else:
            cp("dve", dstT.ap[:, :, dst_cols], bv[:, 0:1024].rearrange("p (c t) -> p c t", c=8), [bk], [dstT])

    def route(tile, h2f):
        tc_ = slice(tile * 128, (tile + 1) * 128)
        bk = bank()
        for c in range(8):
            mm(bk, bk.ap[:, 0:20], h2f[:, c, :], wr_f.ap[:, c, :], [yt, wr_f], start=(c == 0), stop=(c == 7))
        tt("dve", lg.ap, bk.ap[:, 0:20], brb.ap, ALU.add, [bk, brb], [lg])
        G = rt.ap[:, 0, :]
        red(rtm.ap[:, 0:1], lg.ap[:, 0:4], ALU.max, [lg], [rtm])
        ts("dve", rtm.ap[:, 1:2], rtm.ap[:, 0:1], -1.0, None, ALU.mult, None, [rtm], [rtm])
        act(rt.ap[:, 1, :], lg.ap[:, 0:4], AF.Exp, [lg, rtm], [rt, rtm], bias=rtm.ap[:, 1:2], accum=rtm.ap[:, 2:3])
        P.op("dve", lambda e: e.reciprocal(out=rtm.ap[:, 3:4], in_=rtm.ap[:, 2:3]), reads=[rtm], writes=[rtm])
        tt("dve", G, lg.ap[:, 0:4], rtm.ap[:, 0:1].to_broadcast([128, 4]), ALU.is_ge, [lg, rtm], [rt])
        le = lg.ap[:, 4:20].rearrange("p (g e) -> p g e", g=4)
        tt("dve", rt.ap[:, 2:6, :], le, bc3(G, 4), ALU.mult, [lg, rt], [rt])
        red(rt.ap[:, 6, :], rt.ap[:, 2:6, :].rearrange("p g e -> p e g"), ALU.add, [rt], [rt])
        el = rt.ap[:, 6, :]
        red(rtm.ap[:, 4:5], el, ALU.max, [rt], [rtm])
        tt("dve", rt.ap[:, 7, :], el, rtm.ap[:, 4:5].to_broadcast([128, 4]), ALU.is_ge, [rt, rtm], [rt])
        stt(rt.ap[:, 8, :], rt.ap[:, 7, :], -3e30, el, ALU.mult, ALU.add, [rt], [rt])
        red(rtm.ap[:, 5:6], rt.ap[:, 8, :], ALU.max, [rt], [rtm])
        tt("dve", rt.ap[:, 9, :], rt.ap[:, 8, :], rtm.ap[:, 5:6].to_broadcast([128, 4]), ALU.is_ge, [rt, rtm], [rt])
        tt("dve", rtm.ap[:, 6:7], rtm.ap[:, 5:6], rtm.ap[:, 4:5], ALU.subtract, [rtm], [rtm])
        act(rtm.ap[:, 7:8], rtm.ap[:, 6:7], AF.Exp, [rtm], [rtm])
        ts("dve", rtm.ap[:, 8:9], rtm.ap[:, 7:8], 1.0, None, ALU.add, None, [rtm], [rtm])
        P.op("dve", lambda e: e.reciprocal(out=rtm.ap[:, 9:10], in_=rtm.ap[:, 8:9]), reads=[rtm], writes=[rtm])
        tt("dve", rtm.ap[:, 10:11], rtm.ap[:, 7:8], rtm.ap[:, 9:10], ALU.mult, [rtm], [rtm])
        tt("dve", rtm.ap[:, 9:10], rtm.ap[:, 9:10], rtm.ap[:, 3:4], ALU.mult, [rtm], [rtm])
        tt("dve", rtm.ap[:, 10:11], rtm.ap[:, 10:11], rtm.ap[:, 3:4], ALU.mult, [rtm], [rtm])
        ts("dve", rt.ap[:, 10, :], rt.ap[:, 7, :], rtm.ap[:, 9:10], None, ALU.mult, None, [rt, rtm], [rt])
        stt(rt.ap[:, 11, :], rt.ap[:, 9, :], rtm.ap[:, 10:11], rt.ap[:, 10, :], ALU.mult, ALU.add, [rt, rtm], [rt])
        tt("dve", comb.ap[:, tile, :].rearrange("p (g e) -> p g e", g=4), bc3(G, 4),
           rt.ap[:, 11, :].unsqueeze(1).to_broadcast([128, 4, 4]), ALU.mult, [rt], [comb])

    if stage >= 4:
        for tile in ntl:
            tc_ = slice(tile * 128, (tile + 1) * 128)
            ld(acc, acc.ap[:, tile, :], x1_s.ap[tile * 128:(tile + 1) * 128, :], reads=[x1_s])
            rms2(acc.ap[:, tile, :], acc, None, None, None)
            ts("dve", sgt.ap, acc.ap[:, tile, :], nst2.ap[:, 3:4], None, ALU.mult, None, [acc, nst2], [sgt])
            h2f = yt.ap.rearrange("p (c t) -> p c t", c=8)
            for half in range(2):
                bk = bank()
                for c4 in range(4):
                    c = half * 4 + c4
                    tr(bk, bk.ap[:, c4 * 128:(c4 + 1) * 128], sgt.ap[:, c * 128:(c + 1) * 128], ident_f.ap, [sgt, ident_f])
                tt("dve", h2f[:, half * 4:half * 4 + 4, :], bk.ap[:, 0:512].rearrange("p (c t) -> p c t", c=4),
                   g2c.ap[:, half * 4:half * 4 + 4].unsqueeze(2).to_broadcast([128, 4, 128]), ALU.mult, [bk, g2c], [yt])
            cp("pool", h2T.ap[:, :, tc_], h2f, [yt], [h2T])
            route(tile, h2f)
        tgs = []
        cur = []
        for tile in ntl:
            if cur and (tile != cur[-1] + 1 or len(cur) == 4):
                tgs.append(cur)
                cur = []
            cur.append(tile)
        if cur:
            tgs.append(cur)
        for e_ in range(N_EXP):
            wg = weg_b[e_ % 2]
            wu = weu_b[e_ % 2]
            wd = wed_b[e_ % 2]
            ld(stg4, stg2.ap, weg_d[e_].rearrange("(c p) f -> p c f", p=128))
            cp("pool", wg.ap, stg2.ap, [stg4], [wg])
            ld(stg4, stg2.ap, weu_d[e_].rearrange("(c p) f -> p c f", p=128))
            cp("pool", wu.ap, stg2.ap, [stg4], [wu])
            ld(stg4, stg4.ap, wed_d[e_].rearrange("(c p) d -> p c d", p=128))
            cp("dve", wd.ap, stg4.ap, [stg4], [wd])
            for tg in tgs:
                n_ = len(tg) * 128
                cs_ = slice(tg[0] * 128, tg[0] * 128 + n_)
                at = actT[k.pti % 2]
                k.pti += 1
                for fc in range(2):
                    bg_ = bank()
                    bu_ = bank()
                    for c in range(8):
                        mm(bg_, bg_.ap[:, 0:n_], wg.ap[:, c, fc * 128:(fc + 1) * 128], h2T.ap[:, c, cs_], [wg, h2T], start=(c == 0), stop=(c == 7))
                    for c in range(8):
                        mm(bu_, bu_.ap[:, 0:n_], wu.ap[:, c, fc * 128:(fc + 1) * 128], h2T.ap[:, c, cs_], [wu, h2T], start=(c == 0), stop=(c == 7))
                    act(sil.ap[:, fc, 0:n_], bg_.ap[:, 0:n_], AF.Silu, [bg_], [sil])
                    tt("dve", at.ap[:, fc, 0:n_], sil.ap[:, fc, 0:n_], bu_.ap[:, 0:n_], ALU.mult, [sil, bu_], [at])
                for i, tile in enumerate(tg):
                    for dh in range(2):
                        by = bank()
                        for fc in range(2):
                            mm(by, by.ap[:, 0:512], at.ap[:, fc, i * 128:(i + 1) * 128], wd.ap[:, fc, dh * 512:(dh + 1) * 512], [at, wd],
                               start=(fc == 0), stop=(fc == 1))
                        stt(acc.ap[:, tile, dh * 512:(dh + 1) * 512], by.ap[:, 0:512], comb.ap[:, tile, e_:e_ + 1],
                            acc.ap[:, tile, dh * 512:(dh + 1) * 512], ALU.mult, ALU.add, [by, comb, acc], [acc])
        for tile in ntl:
            x2 = acc.ap[:, tile, :]
            to_T(x2, acc, x2T, slice(0, 128))
            ld(pt_f, pt_f.ap, p_d[tile * 128:(tile + 1) * 128, :])
            cp("pool", pt_b.ap, pt_f.ap, [pt_f], [pt_b])
            bk = bank()
            bv = bfv(bk)
            for c in range(2):
                tr(bk, bv[:, c * 128:(c + 1) * 128], pt_b.ap[:, c * 128:(c + 1) * 128], ident_b.ap, [pt_b, ident_b])
            cp("act", pT.ap, bv[:, 0:256].rearrange("p (c t) -> p c t", c=2), [bk], [pT])
            for dh in range(2):
                dsl = slice(dh * 512, (dh + 1) * 512)
                bgt = bank()
                for c in range(8):
                    mm(bgt, bgt.ap[:, 0:512], x2T.ap[:, c, :], wpleg_b.ap[:, c, dsl], [x2T, wpleg_b], start=(c == 0), stop=(c == 7))
                act(sgt.ap[:, dsl], bgt.ap[:, 0:512], AF.Sigmoid, [bgt], [sgt])
                bpe = bank()
                for c in range(2):
                    mm(bpe, bpe.ap[:, 0:512], pT.ap[:, c, :], wple_b.ap[:, c, dsl], [pT, wple_b], start=(c == 0), stop=(c == 1))
                tt("dve", sgt.ap[:, dsl], sgt.ap[:, dsl], bpe.ap[:, 0:512], ALU.mult, [sgt, bpe], [sgt])
            tt("pool", yt.ap, sgt.ap, x2, ALU.add, [sgt, acc], [yt])
            rms2(yt.ap, yt, None, None, None)
            stt(yt.ap, yt.ap, nst2.ap[:, 3:4], fgb.ap, ALU.mult, ALU.mult, [yt, nst2, fgb], [yt])
            st(y_o[tile * 128:(tile + 1) * 128, :], yt.ap, [yt])

    P.emit()
    ph2.close()
    return k


def core_inputs(inp, c, consts=None):
    f = lambda a: np.ascontiguousarray(np.asarray(a))
    consts = consts if consts is not None else host_consts()
    x = np.zeros((NT_ALL * 128, D_MODEL), np.float32)
    p = np.zeros((NT_ALL * 128, PLE_DIM), np.float32)
    x[:SEQ] = np.asarray(inp["x_prompt"])[c]
    p[:SEQ] = np.asarray(inp["p_prompt"])[0, c]
    for b in range(4):
        x[SPOS[b]] = np.asarray(inp["x_sample"])[4 * c + b, 0]
        p[SPOS[b]] = np.asarray(inp["p_sample"])[0, 4 * c + b, 0]
    m = {
        "x": x, "p": p,
        "cache_k": np.asarray(inp["cache_k"]).reshape(2560 * 128, 512),
        "cache_v": np.asarray(inp["cache_v"]).reshape(2560 * 128, 512),
        "page_table": f(np.asarray(inp["page_table"])[4 * c:4 * c + 4]).astype(np.int32),
        "state_wkv": f(np.asarray(inp["state_wkv"])[0, 4 * c:4 * c + 4]),
        "state_shift": f(np.asarray(inp["state_shift"])[0, 4 * c:4 * c + 4]),
        "rel_bias": f(inp["rel_bias"]),
        "final_g": f(inp["final_g"]),
        "r_k": f(np.asarray(inp["r_k"])[0]).reshape(W_R),
    }
    for nm in ("norm1_g", "w_in", "shift_mu", "w0", "w2", "a0", "a2", "g2", "k_k", "k_a", "lnx_g", "lnx_b",
               "w_out", "norm2_g", "w_rg", "b_rg", "w_re", "b_re", "w_eg", "w_eu", "w_ed", "w_ple", "w_pleg"):
        m[nm] = f(np.asarray(inp[nm])[0])
    m.update(consts)
    return m


def assemble(res):
    y_p = np.zeros((8, SEQ, D_MODEL), np.float32)
    y_s = np.zeros((32, 1, D_MODEL), np.float32)
    k_p = np.zeros((1, 8, SEQ, 8, 64), np.float32)
    v_p = np.zeros((1, 8, SEQ, 8, 64), np.float32)
    k_s = np.zeros((1, 32, 1, 8, 64), np.float32)
    v_s = np.zeros((1, 32, 1, 8, 64), np.float32)
    w_p = np.zeros((1, 8, 8, 64, 64), np.float32)
    w_s = np.zeros((1, 32, 8, 64, 64), np.float32)
    s_p = np.zeros((1, 8, RW_COLS), np.float32)
    s_s = np.zeros((1, 32, RW_COLS), np.float32)
    for c, r in enumerate(res):
        y_p[c] = r["y"][:SEQ]
        k_p[0, c] = r["k_new"][:SEQ].reshape(SEQ, 8, 64)
        v_p[0, c] = r["v_new"][:SEQ].reshape(SEQ, 8, 64)
        w_p[0, c] = r["wkv_p"]
        s_p[0, c] = r["shift_p"]
        for b in range(4):
            y_s[4 * c + b, 0] = r["y"][SPOS[b]]
            k_s[0, 4 * c + b, 0] = r["k_new"][SPOS[b]].reshape(8, 64)
            v_s[0, 4 * c + b, 0] = r["v_new"][SPOS[b]].reshape(8, 64)
            w_s[0, 4 * c + b] = r["wkv_s"][b]
            s_s[0, 4 * c + b] = r["shift_s"][b]
    return (y_p, y_s, k_p, v_p, k_s, v_s, w_p, w_s, s_p, s_s)


def kernel(**inputs):
    nc = bass.Bass("TRN2", target_bir_lowering=False)
    with ExitStack() as es:
        k = build(nc, es)
    consts = host_consts()
    in_maps = [core_inputs(inputs, c, consts) for c in range(8)]
    res = run_bass_kernel_spmd(nc, in_maps, core_ids=list(range(8)))
    return assemble(res.results)
```
